# Optimizing a Trainium2 kernel written in Bass

```python
import jax, jax.numpy as jnp
from jax import lax
import numpy as np

D_MODEL = 1024
BATCH = 8
SEQ = 4096
DEPTH = 2

D_PLE = 256
D_CONF = 512
CONF_KERNEL = 31
D_SC = 512
SC_KERNEL = 3
D_FF = 2816
N_EXPERTS = 8
TOP_K = 2
D_EXPERT = 3584
MOE_BLOCK = 256
LN_EPS = 1e-5
DEEPNORM_ALPHA = (2 * DEPTH) ** 0.25
DEEPNORM_BETA = (8 * DEPTH) ** -0.25
N_DENSE = (DEPTH + 1) // 2
N_MOE = DEPTH // 2
IN_COLS = 2 * D_CONF + 3 * D_SC + 2 * D_MODEL
IN_SPLITS = (D_CONF, 2 * D_CONF, 2 * D_CONF + D_SC, 2 * D_CONF + 2 * D_SC,
             2 * D_CONF + 3 * D_SC, 2 * D_CONF + 3 * D_SC + D_MODEL)

kernel_name = "hybrid_conformer_shortconv_gated_moe_deepnorm"


def layer_norm(x, g, b):
    xf = x.astype(jnp.float32)
    mu = jnp.mean(xf, axis=-1, keepdims=True)
    var = jnp.mean(jnp.square(xf - mu), axis=-1, keepdims=True)
    return ((xf - mu) * lax.rsqrt(var + LN_EPS)).astype(x.dtype) * g + b


def causal_depthwise_conv(u, w):
    k, c = w.shape
    return lax.conv_general_dilated(
        u, w[:, None, :].astype(u.dtype), window_strides=(1,), padding=[(k - 1, 0)],
        dimension_numbers=("NWC", "WIO", "NWC"), feature_group_count=c)


def token_mixer(x, w_in, b_in, conf_conv_w, conf_conv_b, conf_ln_g, conf_ln_b,
                w_conf_out, sc_conv_w, w_sc_out, w_o):
    z = jnp.einsum("bsd,dc->bsc", x, w_in) + b_in
    conf_val, conf_gate, sc_b, sc_c, sc_h, gate_a, gate_b = jnp.split(z, IN_SPLITS, axis=-1)
    a = conf_val * jax.nn.sigmoid(conf_gate)
    a = causal_depthwise_conv(a, conf_conv_w) + conf_conv_b
    a = jax.nn.silu(layer_norm(a, conf_ln_g, conf_ln_b))
    y_a = jnp.einsum("bsc,cd->bsd", a, w_conf_out)
    s = causal_depthwise_conv(sc_c * sc_h, sc_conv_w)
    y_b = jnp.einsum("bsc,cd->bsd", sc_b * s, w_sc_out)
    m = jax.nn.sigmoid(gate_a) * y_a + jax.nn.sigmoid(gate_b) * y_b
    return jnp.einsum("bsd,de->bse", m, w_o)


def dense_swiglu(x, w_gate, w_up, w_down):
    h = jax.nn.silu(jnp.einsum("bsd,df->bsf", x, w_gate)) * jnp.einsum("bsd,df->bsf", x, w_up)
    return jnp.einsum("bsf,fd->bsd", h, w_down)


def moe_swiglu(x, w_router, b_router, we_gate, we_up, we_down):
    bsz, seq, d = x.shape
    n_tok = bsz * seq
    xt = x.reshape(n_tok, d)
    logits = jnp.einsum("td,de->te", xt, w_router).astype(jnp.float32) + b_router.astype(jnp.float32)
    top_vals, top_idx = lax.top_k(logits, TOP_K)
    top_w = jax.nn.softmax(top_vals, axis=-1)
    n_assign = n_tok * TOP_K
    e_flat = top_idx.reshape(-1).astype(jnp.int32)
    tok_flat = jnp.repeat(jnp.arange(n_tok, dtype=jnp.int32), TOP_K)
    w_flat = top_w.reshape(-1)
    order = jnp.argsort(e_flat)
    e_sorted = e_flat[order]
    counts = jnp.zeros((N_EXPERTS,), jnp.int32).at[e_flat].add(1)
    padded = (counts + MOE_BLOCK - 1) // MOE_BLOCK * MOE_BLOCK
    pad_end = jnp.cumsum(padded)
    pad_start = pad_end - padded
    start = jnp.cumsum(counts) - counts
    rank = jnp.arange(n_assign, dtype=jnp.int32) - start[e_sorted]
    dest = pad_start[e_sorted] + rank
    n_rows = (-(-n_assign // MOE_BLOCK)) * MOE_BLOCK + N_EXPERTS * MOE_BLOCK
    n_blocks = n_rows // MOE_BLOCK
    row_tok = jnp.zeros((n_rows,), jnp.int32).at[dest].set(tok_flat[order])
    row_w = jnp.zeros((n_rows,), jnp.float32).at[dest].set(w_flat[order])
    block_e = jnp.minimum(
        jnp.searchsorted(pad_end, jnp.arange(n_blocks, dtype=jnp.int32) * MOE_BLOCK, side="right"),
        N_EXPERTS - 1).astype(jnp.int32)
    xb = xt[row_tok].reshape(n_blocks, MOE_BLOCK, d)

    def expert_block(args):
        xblk, e = args
        h = jax.nn.silu(xblk @ we_gate[e]) * (xblk @ we_up[e])
        return h @ we_down[e]

    yb = lax.map(expert_block, (xb, block_e))
    y_rows = yb.reshape(n_rows, d) * row_w[:, None].astype(x.dtype)
    out = jnp.zeros((n_tok, d), x.dtype).at[row_tok].add(y_rows)
    return out.reshape(bsz, seq, d)


def setup_inputs(seed: int = 0) -> dict:
    key = jax.random.key(seed)
    ks = iter(jax.random.split(key, 32))

    def nrm(shape, scale):
        return jax.random.normal(next(ks), shape, jnp.float32) * scale

    L = DEPTH
    return {
        "x": nrm((BATCH, SEQ, D_MODEL), 1.0),
        "p": nrm((DEPTH, BATCH, SEQ, D_PLE), 1.0),
        "w_in": nrm((L, D_MODEL, IN_COLS), D_MODEL ** -0.5),
        "b_in": nrm((L, IN_COLS), 0.02),
        "conf_conv_w": nrm((L, CONF_KERNEL, D_CONF), CONF_KERNEL ** -0.5),
        "conf_conv_b": nrm((L, D_CONF), 0.02),
        "conf_ln_g": 1.0 + nrm((L, D_CONF), 0.02),
        "conf_ln_b": nrm((L, D_CONF), 0.02),
        "w_conf_out": nrm((L, D_CONF, D_MODEL), D_CONF ** -0.5),
        "sc_conv_w": nrm((L, SC_KERNEL, D_SC), SC_KERNEL ** -0.5),
        "w_sc_out": nrm((L, D_SC, D_MODEL), D_SC ** -0.5),
        "w_o": nrm((L, D_MODEL, D_MODEL), D_MODEL ** -0.5 * DEEPNORM_BETA),
        "ln1_g": 1.0 + nrm((L, D_MODEL), 0.02),
        "ln1_b": nrm((L, D_MODEL), 0.02),
        "w_ff_gate": nrm((N_DENSE, D_MODEL, D_FF), D_MODEL ** -0.5),
        "w_ff_up": nrm((N_DENSE, D_MODEL, D_FF), D_MODEL ** -0.5),
        "w_ff_down": nrm((N_DENSE, D_FF, D_MODEL), D_FF ** -0.5 * DEEPNORM_BETA),
        "w_router": nrm((N_MOE, D_MODEL, N_EXPERTS), D_MODEL ** -0.5),
        "b_router": nrm((N_MOE, N_EXPERTS), 0.01),
        "we_gate": nrm((N_MOE, N_EXPERTS, D_MODEL, D_EXPERT), D_MODEL ** -0.5),
        "we_up": nrm((N_MOE, N_EXPERTS, D_MODEL, D_EXPERT), D_MODEL ** -0.5),
        "we_down": nrm((N_MOE, N_EXPERTS, D_EXPERT, D_MODEL), D_EXPERT ** -0.5 * DEEPNORM_BETA),
        "w_ple_gate": nrm((L, D_MODEL, D_MODEL), D_MODEL ** -0.5),
        "b_ple_gate": nrm((L, D_MODEL), 0.02),
        "w_ple_proj": nrm((L, D_PLE, D_MODEL), D_PLE ** -0.5 * DEEPNORM_BETA),
        "ln2_g": 1.0 + nrm((L, D_MODEL), 0.02),
        "ln2_b": nrm((L, D_MODEL), 0.02),
    }


def reference(x, p, w_in, b_in, conf_conv_w, conf_conv_b, conf_ln_g, conf_ln_b,
              w_conf_out, sc_conv_w, w_sc_out, w_o, ln1_g, ln1_b,
              w_ff_gate, w_ff_up, w_ff_down, w_router, b_router, we_gate, we_up, we_down,
              w_ple_gate, b_ple_gate, w_ple_proj, ln2_g, ln2_b):
    for i in range(DEPTH):
        mix = token_mixer(x, w_in[i], b_in[i], conf_conv_w[i], conf_conv_b[i], conf_ln_g[i],
                          conf_ln_b[i], w_conf_out[i], sc_conv_w[i], w_sc_out[i], w_o[i])
        x = layer_norm(DEEPNORM_ALPHA * x + mix, ln1_g[i], ln1_b[i])
        j = i // 2
        if i % 2 == 0:
            ffn = dense_swiglu(x, w_ff_gate[j], w_ff_up[j], w_ff_down[j])
        else:
            ffn = moe_swiglu(x, w_router[j], b_router[j], we_gate[j], we_up[j], we_down[j])
        ple = jax.nn.sigmoid(jnp.einsum("bsd,de->bse", x, w_ple_gate[i]) + b_ple_gate[i]) \
            * jnp.einsum("bsq,qd->bsd", p[i], w_ple_proj[i])
        x = layer_norm(DEEPNORM_ALPHA * x + ffn + ple, ln2_g[i], ln2_b[i])
    return x
```

```python
import numpy as np
from contextlib import ExitStack
from functools import partial
import concourse.bass as bass
import concourse.mybir as mybir
from concourse.bass_utils import run_bass_kernel_spmd

F32 = mybir.dt.float32
BF16 = mybir.dt.bfloat16
AF = mybir.ActivationFunctionType
ALU = mybir.AluOpType
AX = mybir.AxisListType

D = 1024
SEQ = 4096
NCORES = 8
TB = 1024
NBLK = SEQ // TB
SUB = 512
NSUB = TB // SUB
NT = TB // 128
DPLE = 256
KCONF = 31
KSC = 3
DFF = 2816
NE = 8
DEXP = 3584
INC = 4608
ALPHA = float(4 ** 0.25)
EPS = 1e-5
HALO = 32
NCOLV = 184
NSLOT = 6
NPS = 6
NTMP = 5
CAP = 384
NJ = CAP // 128


class Op:
    __slots__ = ("eng", "fn", "dma_sem", "dma_cnt", "idx", "epoch", "waits", "signal", "sigcnt")


class Prog:
    ENGS = ("pe", "act", "dve", "pool", "sp")

    def __init__(self):
        self.ops = {e: [] for e in self.ENGS}
        self.last_w = {}
        self.readers = {}
        self.waited = {e: {} for e in self.ENGS}
        self.waited_dma = {e: {} for e in self.ENGS}
        self.dma_counts = {}
        self.epoch = 0

    def add(self, eng, fn, reads=(), writes=(), dma=None):
        op = Op()
        op.eng = eng
        op.fn = fn
        op.dma_sem = dma
        op.dma_cnt = 0
        op.epoch = self.epoch
        op.waits = []
        op.signal = False
        op.sigcnt = 0
        op.idx = len(self.ops[eng])
        if dma is not None:
            self.dma_counts[dma] = self.dma_counts.get(dma, 0) + 16
            op.dma_cnt = self.dma_counts[dma]
        deps = []
        for k in reads:
            w = self.last_w.get(k)
            if w is not None:
                deps.append(w)
        for k in writes:
            w = self.last_w.get(k)
            if w is not None:
                deps.append(w)
            deps.extend(self.readers.get(k, ()))
        for d in deps:
            if d.dma_sem is not None:
                cur = self.waited_dma[eng].get(d.dma_sem, 0)
                if d.dma_cnt > cur:
                    self.waited_dma[eng][d.dma_sem] = d.dma_cnt
                    op.waits.append(("dma", d.dma_sem, d.dma_cnt))
            else:
                if d.eng == eng and eng == "pe":
                    continue
                cur = self.waited[eng].get(d.eng, -1)
                if d.idx > cur:
                    self.waited[eng][d.eng] = d.idx
                    d.signal = True
                    op.waits.append(("eng", d))
        for k in writes:
            self.last_w[k] = op
            self.readers[k] = []
        for k in reads:
            if k not in writes:
                self.readers.setdefault(k, []).append(op)
        self.ops[eng].append(op)
        return op

    def finalize(self):
        self.max_epoch = 0
        for e in self.ENGS:
            cnt = {}
            for op in self.ops[e]:
                if op.signal:
                    cnt[op.epoch] = cnt.get(op.epoch, 0) + 1
                    op.sigcnt = cnt[op.epoch]
                self.max_epoch = max(self.max_epoch, op.epoch)

    def emit(self, nc, es):
        self.finalize()
        nep = self.max_epoch + 1
        esem = {e: [es.enter_context(nc.semaphore(f"s_{e}_{i}")) for i in range(nep)] for e in self.ENGS}
        dsem = {n: es.enter_context(nc.semaphore(f"d_{n}")) for n in self.dma_counts}
        block = es.enter_context(nc.Block())

        def run(e, h):
            for op in self.ops[e]:
                for w in op.waits:
                    if w[0] == "dma":
                        h.wait_ge(dsem[w[1]], w[2])
                    else:
                        d = w[1]
                        h.wait_ge(esem[d.eng][d.epoch], d.sigcnt)
                if op.fn is None:
                    continue
                inst = op.fn(h)
                if op.dma_sem is not None:
                    inst.then_inc(dsem[op.dma_sem], 16)
                elif op.signal:
                    inst.then_inc(esem[e][op.epoch], 1)

        block.tensor(partial(run, "pe"))
        block.scalar(partial(run, "act"))
        block.vector(partial(run, "dve"))
        block.gpsimd(partial(run, "pool"))
        block.sync(partial(run, "sp"))


def f_mm(out, pairs):
    def fn(pe):
        n = len(pairs)
        inst = None
        for i, (l, r) in enumerate(pairs):
            inst = pe.matmul(out, l, r, start=(i == 0), stop=(i == n - 1))
        return inst
    return fn


def f_tr(outs_ins, ident):
    def fn(pe):
        inst = None
        for o, i in outs_ins:
            inst = pe.transpose(o, i, ident)
        return inst
    return fn


def f_act(out, in_, func, bias=None, scale=None):
    def fn(e):
        kw = {}
        if bias is not None:
            kw["bias"] = bias
        if scale is not None:
            kw["scale"] = scale
        return e.activation(out=out, in_=in_, func=func, **kw)
    return fn


def f_stt(out, in0, scalar, in1, op0, op1):
    return lambda e: e.scalar_tensor_tensor(out=out, in0=in0, scalar=scalar, in1=in1, op0=op0, op1=op1)


def f_tt(out, in0, in1, op):
    return lambda e: e.tensor_tensor(out=out, in0=in0, in1=in1, op=op)


def f_ts(out, in0, s1, s2, op0, op1=None):
    if op1 is None:
        return lambda e: e.tensor_scalar(out=out, in0=in0, scalar1=s1, scalar2=None, op0=op0)
    return lambda e: e.tensor_scalar(out=out, in0=in0, scalar1=s1, scalar2=s2, op0=op0, op1=op1)


def f_recip(ap):
    return lambda e: e.reciprocal(out=ap, in_=ap)


def f_copy(out, in_):
    return lambda e: e.tensor_copy(out=out, in_=in_)


def f_memset(ap, v):
    return lambda e: e.memset(ap, v)


def f_dma(out, in_):
    return lambda e: e.dma_start(out=out, in_=in_)


def build_nc(n_blk=NBLK, layers=(0, 1)):
    nc = bass.Bass("TRN2", target_bir_lowering=False)

    def din(name, shape):
        return nc.dram_tensor(name, list(shape), F32, kind="ExternalInput").ap()

    x = din("x", [SEQ, D])
    p_in = din("p", [2, SEQ, DPLE])
    w_in = din("w_in", [2, D, INC])
    w_conf_out = din("w_conf_out", [2, 512, D])
    w_sc_out = din("w_sc_out", [2, 512, D])
    w_o = din("w_o", [2, D, D])
    w_ff_gate = din("w_ff_gate", [1, D, DFF])
    w_ff_up = din("w_ff_up", [1, D, DFF])
    w_ff_down = din("w_ff_down", [1, DFF, D])
    w_router = din("w_router", [1, D, NE])
    b_router = din("b_router", [1, NE])
    we_gate = din("we_gate", [1, NE, D, DEXP])
    we_up = din("we_up", [1, NE, D, DEXP])
    we_down = din("we_down", [1, NE, DEXP, D])
    w_ple_gate = din("w_ple_gate", [2, D, D])
    b_ple_gate = din("b_ple_gate", [1, 2 * D])
    w_ple_proj = din("w_ple_proj", [2, DPLE, D])
    ln1_g = din("ln1_g", [2, D])
    ln1_b = din("ln1_b", [2, D])
    ln2_g = din("ln2_g", [2, D])
    ln2_b = din("ln2_b", [2, D])
    colv_d = din("colv", [128, 2 * NCOLV])
    ident_d = din("ident", [128, 128])
    iota_d = din("iota", [128, 512])
    triu_d = din("triu", [128, 128])
    out = nc.dram_tensor("out", [SEQ, D], F32, kind="ExternalOutput").ap()
    sg = nc.dram_tensor("scr_g", [NE, D, DEXP], BF16, kind="Internal").ap()
    su = nc.dram_tensor("scr_u", [NE, D, DEXP], BF16, kind="Internal").ap()
    sd = nc.dram_tensor("scr_d", [NE, DEXP, D], BF16, kind="Internal").ap()

    es = ExitStack()
    P = Prog()

    def sb(name, shape, dt):
        return es.enter_context(nc.sbuf_tensor(name, list(shape), dt))

    xres = sb("xres", [128, NT, D], F32)
    xT = sb("xT", [128, 8, TB], BF16)
    xtok = [sb(f"xtok{i}", [128, D], BF16) for i in range(2)]
    cin = sb("cin", [128, 4, HALO + TB], F32)
    a2T = sb("a2T", [128, 4, TB], BF16)
    bT = sb("bT", [128, 4, TB], BF16)
    mT = sb("mT", [128, 8, TB], BF16)
    cacc = sb("cacc", [128, 4, SUB], F32)
    sqt = sb("sqt", [128, 4, SUB], F32)
    tmps = [sb(f"tmp{i}", [128, SUB], F32) for i in range(NTMP)]
    hT = [sb(f"hT{i}", [128, 4, SUB], BF16) for i in range(2)]
    wsl = [sb(f"wsl{i}", [128, 4096], BF16) for i in range(NSLOT)]
    lng = sb("lng", [128, D], F32)
    lnb = sb("lnb", [128, D], F32)
    ptok = [sb(f"ptok{i}", [128, DPLE], BF16) for i in range(2)]
    pT = sb("pT", [128, 2, TB], BF16)
    colv = sb("colv_s", [128, 2 * NCOLV], F32)
    halo_a = [sb(f"halo_a{l}", [128, 4, HALO], F32) for l in range(2)]
    halo_c = [sb(f"halo_c{l}", [128, 4, HALO], F32) for l in range(2)]
    ident = sb("ident_s", [128, 128], BF16)
    ones_f = sb("ones_f", [128, 128], F32)
    ones_row = sb("ones_row", [1, 128], BF16)
    wr = sb("wr", [128, 8, NE], BF16)
    brow_r = sb("brow_r", [1, NE], BF16)
    brow_pg = sb("brow_pg", [1, D], BF16)
    gate = sb("gate", [128, NT, NE], F32)
    sst = [sb(f"sst{i}", [128, 48], F32) for i in range(4)]
    eps_t = sb("eps_t", [128, 1], F32)
    lnst = sb("lnst", [128, NT, 16], F32)
    lgt = sb("lgt", [128, NT * NE], F32)
    iota_t = sb("iota_t", [128, 512], F32)
    triu = sb("triu_s", [128, 128], BF16)
    ones_b = sb("ones_b", [128, 128], BF16)
    msk = sb("msk", [128, NT, NE], BF16)
    mskf = sb("mskf", [128, NT, NE], F32)
    rk = sb("rk", [128, NT, NE], F32)
    cacc_b = cacc[:, :, :].bitcast(BF16)
    cin_b = cin[:, :, :].bitcast(BF16)
    halo_ab = [h[:, :, :].bitcast(BF16) for h in halo_a]
    dgv = [h[:, :, :].rearrange("p a (b q) -> p (a b) q", q=128) for h in hT]
    sqt_b = sqt[:, :, :].bitcast(BF16)
    lng_b = lng[:, :].bitcast(BF16)
    lnb_b = lnb[:, :].bitcast(BF16)

    ps = [es.enter_context(nc.psum_tensor(f"ps{i}", [128, SUB], F32)) for i in range(NPS)]
    pst = [es.enter_context(nc.psum_tensor(f"pst{i}", [128, 1024], BF16)) for i in range(2)]

    cnt = {"ps": 0, "pst": 0, "tmp": 0, "w": 0, "sst": 0, "xtok": 0, "ptok": 0, "h": 0}

    def rr(name, n):
        i = cnt[name] % n
        cnt[name] += 1
        return i

    def ps_next():
        i = rr("ps", NPS)
        return ps[i], ("ps", i)

    def pst_next():
        i = rr("pst", 2)
        return pst[i], ("pst", i)

    def tmp_next():
        i = rr("tmp", NTMP)
        return tmps[i], ("tmp", i)

    conv_units = []
    if 1 in layers:
        for e in range(NE):
            for (dst, srcw, rows) in ((sg, we_gate, D), (su, we_up, D), (sd, we_down, DEXP)):
                conv_units.append((e, dst, srcw, rows))
    conv_state = {"calls": 0}

    def emit_conv_unit():
        if not conv_units:
            return
        e, dst, srcw, rows = conv_units.pop(0)
        q = rows // 4
        for j in range(4):
            P.add("pool", f_dma(dst[e, j * q:(j + 1) * q, :], srcw[0, e, j * q:(j + 1) * q, :]),
                  writes=[("scr", e, id(dst), j)], dma=f"cv{e}_{rows}_{id(dst) % 9973}")

    def load_w(src_ap, k, c):
        i = rr("w", NSLOT)
        view = wsl[i][:, 0:k * c].rearrange("p (k c) -> p k c", k=k)
        P.add("pool", f_dma(view, src_ap.rearrange("(k p) c -> p k c", p=128)),
              writes=[("w", i)], dma=f"w{i}")
        conv_state["calls"] += 1
        if conv_state["calls"] % 2 == 0:
            emit_conv_unit()
        return view, ("w", i)

    def load_w_scr(src_ap, k, c, e, dst):
        while any(u[0] == e for u in conv_units):
            emit_conv_unit()
        i = rr("w", NSLOT)
        view = wsl[i][:, 0:k * c].rearrange("p (k c) -> p k c", k=k)
        P.add("sp", f_dma(view, src_ap.rearrange("(k p) c -> p k c", p=128)),
              reads=[("scr", e, id(dst), j) for j in range(4)], writes=[("w", i)], dma=f"w{i}")
        return view, ("w", i)

    P.add("sp", f_dma(colv[:, :], colv_d[:, :]), writes=[("colv",)], dma="c0")
    P.add("pool", f_dma(ident[:, :], ident_d[:, :]), writes=[("ident",)], dma="c1")
    P.add("pool", f_dma(wr[:, :, :], w_router[0].rearrange("(k p) e -> p k e", p=128)),
          writes=[("wr",)], dma="c2")
    P.add("pool", f_dma(brow_r[:, :], b_router[0:1, :]), writes=[("brow_r",)], dma="c3")
    P.add("dve", f_memset(ones_f[:, :], 1.0), writes=[("ones_f",)])
    P.add("dve", f_memset(ones_row[:, :], 1.0), writes=[("ones_row",)])
    P.add("dve", f_memset(eps_t[:, :], EPS), writes=[("eps",)])
    P.add("dve", f_memset(ones_b[:, :], 1.0), writes=[("ones_b",)])
    P.add("sp", f_dma(iota_t[:, :], iota_d[:, :]), writes=[("iota",)], dma="c5")
    P.add("pool", f_dma(triu[:, :], triu_d[:, :]), writes=[("triu",)], dma="c6")

    def cv(l, j):
        return colv[:, l * NCOLV + j:l * NCOLV + j + 1]

    xT_keys = lambda s: [("xT", s * 4 + i) for i in range(4)]

    def transposes_to(src_tok, src_key, n_k, dst, dst_key, tt):
        pt, kpt = pst_next()
        P.add("pe", f_tr([(pt[:, k * 128:(k + 1) * 128], src_tok[:, k * 128:(k + 1) * 128]) for k in range(n_k)],
                         ident[:, :]),
              reads=[src_key, ("ident",)], writes=[kpt])
        P.add("act", f_act(dst[:, 0:n_k, tt * 128:(tt + 1) * 128],
                           pt[:, 0:n_k * 128].rearrange("p (k t) -> p k t", k=n_k), AF.Copy),
              reads=[kpt], writes=[dst_key])

    def layer_norm_all(to_xT, keep_tok=False):
        ln_group(range(NT), to_xT, keep_tok)

    def ln_group(tiles, to_xT, keep_tok=False):
        ln_compute(tiles, keep_tok and to_xT)
        if to_xT:
            ln_xpose(tiles, keep_tok)

    def ln_compute(tiles, keep_tok=False):
        tiles = list(tiles)
        xk = lambda tt: [("xres", tt, 0), ("xres", tt, 1)]
        st = lambda tt, a, b_: lnst[:, tt, a:b_]
        ks = lambda tt, i: ("lnst", tt, i)
        for tt in tiles:
            P.add("dve", partial(lambda e, tt: e.bn_stats(out=lnst[:, tt, 0:6], in_=xres[:, tt, 0:512]), tt=tt),
                  reads=[xk(tt)[0]], writes=[ks(tt, 0)])
            P.add("dve", partial(lambda e, tt: e.bn_stats(out=lnst[:, tt, 6:12], in_=xres[:, tt, 512:1024]), tt=tt),
                  reads=[xk(tt)[1]], writes=[ks(tt, 1)])
            P.add("dve", partial(lambda e, tt: e.bn_aggr(out=lnst[:, tt, 12:14], in_=lnst[:, tt, 0:12]), tt=tt),
                  reads=[ks(tt, 0), ks(tt, 1)], writes=[ks(tt, 2)])
        for tt in tiles:
            P.add("act", f_act(st(tt, 14, 15), st(tt, 13, 14), AF.Sqrt, bias=eps_t[:, 0:1]),
                  reads=[ks(tt, 2), ("eps",)], writes=[ks(tt, 3)])
        for tt in tiles:
            P.add("dve", f_recip(st(tt, 14, 15)), reads=[ks(tt, 3)], writes=[ks(tt, 3)])
        for tt in tiles:
            P.add("dve", f_stt(xres[:, tt, :], xres[:, tt, :], st(tt, 12, 13), lng[:, :], ALU.subtract, ALU.mult),
                  reads=xk(tt) + [ks(tt, 2), ("lng",)], writes=xk(tt))
            P.add("dve", f_stt(xres[:, tt, :], xres[:, tt, :], st(tt, 14, 15), lnb[:, :], ALU.mult, ALU.add),
                  reads=xk(tt) + [ks(tt, 3), ("lnb",)], writes=xk(tt))
        if keep_tok:
            for tt in tiles:
                P.add("act", f_act(mT[:, tt, :], xres[:, tt, :], AF.Copy), reads=xk(tt), writes=[("m", tt, 0), ("m", tt, 1)])

    def ln_xpose(tiles, keep_tok=False):
        xk = lambda tt: [("xres", tt, 0), ("xres", tt, 1)]
        for tt in tiles:
            if keep_tok:
                dk = [("m", tt, 0), ("m", tt, 1)]
                pt, kpt = pst_next()
                P.add("pe", f_tr([(pt[:, k * 128:(k + 1) * 128], mT[:, tt, k * 128:(k + 1) * 128]) for k in range(8)], ident[:, :]),
                      reads=dk + [("ident",)], writes=[kpt])
                P.add("act", f_act(xT[:, :, tt * 128:(tt + 1) * 128], pt[:, :].rearrange("p (k t) -> p k t", k=8), AF.Copy),
                      reads=[kpt], writes=[("xT", tt)])
            else:
                i = rr("xtok", 2)
                P.add("act", f_act(xtok[i][:, :], xres[:, tt, :], AF.Copy), reads=xk(tt), writes=[("xtok", i)])
                transposes_to(xtok[i], ("xtok", i), 8, xT, ("xT", tt), tt)

    def load_ln(g_d, b_d, l):
        P.add("sp", f_dma(lng[:, :], g_d[l:l + 1, :].partition_broadcast(128)), writes=[("lng",)], dma="lng")
        P.add("sp", f_dma(lnb[:, :], b_d[l:l + 1, :].partition_broadcast(128)), writes=[("lnb",)], dma="lnb")

    def set_halo(b, halo):
        hk = [("cin", c, "h") for c in range(4)]
        if b == 0:
            P.add("dve", f_memset(cin[:, :, 0:HALO], 0.0), writes=hk)
        else:
            P.add("dve", f_copy(cin[:, :, 0:HALO], halo[:, :, :]), reads=[("halo", id(halo))], writes=hk)

    def save_halo(halo):
        P.add("dve", f_copy(halo[:, :, :], cin[:, :, TB:TB + HALO]),
              reads=[("cin", c, NSUB - 1) for c in range(4)], writes=[("halo", id(halo))])

    def cin_keys(c, s):
        return [("cin", c, "h" if s == 0 else s - 1), ("cin", c, s)]

    sl = lambda s: slice(s * SUB, (s + 1) * SUB)
    csl = lambda s: slice(HALO + s * SUB, HALO + (s + 1) * SUB)

    def mixer_A(l, b, post=None):
        wv, kwv = load_w(w_in[l, :, 0:512], 8, 512)
        wg, kwg = load_w(w_in[l, :, 512:1024], 8, 512)
        ak = lambda c: [("cin", c, "h"), ("cin", c, 0)]
        if b == 0:
            P.add("dve", f_memset(cin_b[:, :, 0:HALO], 0.0), writes=[k for c in range(4) for k in ak(c)])
        else:
            P.add("dve", f_copy(cin_b[:, :, 0:HALO], halo_ab[l][:, :, 0:HALO]), reads=[("halo", id(halo_a[l]))],
                  writes=[k for c in range(4) for k in ak(c)])
        for s in range(NSUB):
            for c in range(4):
                pv, kpv = ps_next()
                P.add("pe", f_mm(pv[:, :], [(wv[:, k, c * 128:(c + 1) * 128], xT[:, k, sl(s)]) for k in range(8)]),
                      reads=[kwv] + xT_keys(s), writes=[kpv])
                pg, kpg = ps_next()
                P.add("pe", f_mm(pg[:, :], [(wg[:, k, c * 128:(c + 1) * 128], xT[:, k, sl(s)]) for k in range(8)]),
                      reads=[kwg] + xT_keys(s), writes=[kpg])
                tm, ktm = tmp_next()
                P.add("act", f_act(tm[:, :], pg[:, :], AF.Sigmoid, bias=cv(l, 4 + c)), reads=[kpg, ("colv",)], writes=[ktm])
                P.add("dve", f_stt(cin_b[:, c, csl(s)], pv[:, :], cv(l, c), tm[:, :], ALU.add, ALU.mult),
                      reads=[kpv, ktm, ("colv",)], writes=ak(c))
                for f in (post or {}).get((s, c), ()):
                    f()

    def mixer_rest(l, b, postB=None):
        ak = lambda c: [("cin", c, "h"), ("cin", c, 0)]
        def conv_mm(s):
            banks = []
            for c in range(4):
                pc_, kpc_ = ps_next()
                banks.append((pc_, kpc_))
                for hi, (j0, nj) in enumerate(((0, 16), (16, KCONF - 16))):
                    wbase = l * NCOLV + 36 + c * KCONF + j0
                    hk = [("h", hi, q) for q in range(4)]
                    P.add("dve", f_tt(dgv[hi][:, 0:nj, :], ident[:, :].unsqueeze(1).to_broadcast([128, nj, 128]),
                                      colv[:, wbase:wbase + nj].unsqueeze(2).to_broadcast([128, nj, 128]), ALU.mult),
                          reads=[("ident",), ("colv",)], writes=hk)

                    def fn(pe, c=c, s=s, hi=hi, j0=j0, nj=nj, pc_=pc_):
                        inst = None
                        for jj in range(nj):
                            j = j0 + jj
                            inst = pe.matmul(pc_[:, :], dgv[hi][:, jj, :], cin_b[:, c, 2 + j + s * SUB:2 + j + s * SUB + SUB],
                                             start=(j == 0), stop=(j == KCONF - 1))
                        return inst
                    P.add("pe", fn, reads=hk + ak(c), writes=[kpc_])
            return banks

        def conv_evac(banks):
            for c, (pc_, kpc_) in enumerate(banks):
                P.add("act", f_act(cacc[:, c, :], pc_[:, :], AF.Identity, bias=cv(l, 160 + c)),
                      reads=[kpc_, ("colv",)], writes=[("cacc", c)])
            for c in range(4):
                P.add("act", f_act(sqt[:, c, :], cacc[:, c, :], AF.Square), reads=[("cacc", c)], writes=[("sqt", c)])

        def ln_sub(s):
            p1, kp1 = ps_next()
            P.add("pe", f_mm(p1[:, :], [(ones_f[:, :], cacc[:, c, :]) for c in range(4)]),
                  reads=[("ones_f",)] + [("cacc", c) for c in range(4)], writes=[kp1])
            p2, kp2 = ps_next()
            P.add("pe", f_mm(p2[:, :], [(ones_f[:, :], sqt[:, c, :]) for c in range(4)]),
                  reads=[("ones_f",)] + [("sqt", c) for c in range(4)], writes=[kp2])
            tmean, kmean = tmp_next()
            tmsq, kmsq = tmp_next()
            P.add("act", f_act(tmean[:, :], p1[:, :], AF.Copy, scale=1.0 / 512), reads=[kp1], writes=[kmean])
            P.add("act", f_act(tmsq[:, :], p1[:, :], AF.Square, scale=1.0 / 512), reads=[kp1], writes=[kmsq])
            P.add("dve", f_stt(tmsq[:, :], p2[:, :], 1.0 / 512, tmsq[:, :], ALU.mult, ALU.subtract),
                  reads=[kp2, kmsq], writes=[kmsq])
            P.add("act", f_act(tmsq[:, :], tmsq[:, :], AF.Sqrt, bias=eps_t[:, 0:1]), reads=[kmsq, ("eps",)], writes=[kmsq])
            P.add("dve", f_recip(tmsq[:, :]), reads=[kmsq], writes=[kmsq])
            for c in range(4):
                P.add("dve", f_stt(cacc[:, c, :], cacc[:, c, :], 1.0, tmean[:, :], ALU.mult, ALU.subtract),
                      reads=[("cacc", c), kmean], writes=[("cacc", c)])
            for c in range(4):
                P.add("dve", f_stt(cacc[:, c, :], cacc[:, c, :], 1.0, tmsq[:, :], ALU.mult, ALU.mult),
                      reads=[("cacc", c), kmsq], writes=[("cacc", c)])
            for c in range(4):
                P.add("act", f_act(a2T[:, c, sl(s)], cacc[:, c, :], AF.Silu, bias=cv(l, 168 + c), scale=cv(l, 164 + c)),
                      reads=[("cacc", c), ("colv",)], writes=[("a2", c, s)])
            for f in (postB or {}).get(s, ()):
                f()

        banks0 = conv_mm(0)
        conv_evac(banks0)
        banks1 = conv_mm(1)
        P.add("dve", f_copy(halo_ab[l][:, :, 0:HALO], cin_b[:, :, TB:TB + HALO]),
              reads=[k for c in range(4) for k in ak(c)], writes=[("halo", id(halo_a[l]))])
        ln_sub(0)
        conv_evac(banks1)
        wb, kwb = load_w(w_in[l, :, 1024:1536], 8, 512)
        wc, kwc = load_w(w_in[l, :, 1536:2048], 8, 512)
        wh, kwh = load_w(w_in[l, :, 2048:2560], 8, 512)
        set_halo(b, halo_c[l])
        for s in range(NSUB):
            for c in range(4):
                pb, kpb = ps_next()
                P.add("pe", f_mm(pb[:, :], [(wb[:, k, c * 128:(c + 1) * 128], xT[:, k, sl(s)]) for k in range(8)]),
                      reads=[kwb] + xT_keys(s), writes=[kpb])
                ph, kph = ps_next()
                P.add("pe", f_mm(ph[:, :], [(wh[:, k, c * 128:(c + 1) * 128], xT[:, k, sl(s)]) for k in range(8)]),
                      reads=[kwh] + xT_keys(s), writes=[kph])
                pc, kpc = ps_next()
                P.add("pe", f_mm(pc[:, :], [(wc[:, k, c * 128:(c + 1) * 128], xT[:, k, sl(s)]) for k in range(8)]),
                      reads=[kwc] + xT_keys(s), writes=[kpc])
                P.add("act", f_act(bT[:, c, sl(s)], pb[:, :], AF.Identity, bias=cv(l, 8 + c)),
                      reads=[kpb, ("colv",)], writes=[("b", c, s)])
                tm, ktm = tmp_next()
                P.add("act", f_act(tm[:, :], ph[:, :], AF.Identity, bias=cv(l, 16 + c)), reads=[kph, ("colv",)], writes=[ktm])
                P.add("dve", f_stt(cin[:, c, csl(s)], pc[:, :], cv(l, 12 + c), tm[:, :], ALU.add, ALU.mult),
                      reads=[kpc, ktm, ("colv",)], writes=[("cin", c, s)])
        ln_sub(1)
        for s in range(NSUB):
            for j in range(KSC):
                for c in range(4):
                    win = cin[:, c, 30 + j + s * SUB:30 + j + s * SUB + SUB]
                    wcol = cv(l, 172 + c * KSC + j)
                    if j == 0:
                        P.add("dve", f_ts(cacc[:, c, :], win, wcol, None, ALU.mult),
                              reads=cin_keys(c, s) + [("colv",)], writes=[("cacc", c)])
                    else:
                        P.add("dve", f_stt(cacc[:, c, :], win, wcol, cacc[:, c, :], ALU.mult, ALU.add),
                              reads=cin_keys(c, s) + [("cacc", c)], writes=[("cacc", c)])
            for c in range(4):
                P.add("dve", f_tt(bT[:, c, sl(s)], cacc[:, c, :], bT[:, c, sl(s)], ALU.mult),
                      reads=[("cacc", c), ("b", c, s)], writes=[("b", c, s)])
        save_halo(halo_c[l])
        for hf in range(2):
            wa, kwa = load_w(w_conf_out[l, :, hf * 512:(hf + 1) * 512], 4, 512)
            wbo, kwbo = load_w(w_sc_out[l, :, hf * 512:(hf + 1) * 512], 4, 512)
            wga, kwga = load_w(w_in[l, :, 2560 + hf * 512:2560 + (hf + 1) * 512], 8, 512)
            wgb, kwgb = load_w(w_in[l, :, 3584 + hf * 512:3584 + (hf + 1) * 512], 8, 512)
            for s in range(NSUB):
                for dq in range(4):
                    dc = hf * 4 + dq
                    q = slice(dq * 128, (dq + 1) * 128)
                    pga, kpga = ps_next()
                    P.add("pe", f_mm(pga[:, :], [(wga[:, k, q], xT[:, k, sl(s)]) for k in range(8)]),
                          reads=[kwga] + xT_keys(s), writes=[kpga])
                    pya, kpya = ps_next()
                    P.add("pe", f_mm(pya[:, :], [(wa[:, k, q], a2T[:, k, sl(s)]) for k in range(4)]),
                          reads=[kwa] + [("a2", k, s) for k in range(4)], writes=[kpya])
                    pgb, kpgb = ps_next()
                    P.add("pe", f_mm(pgb[:, :], [(wgb[:, k, q], xT[:, k, sl(s)]) for k in range(8)]),
                          reads=[kwgb] + xT_keys(s), writes=[kpgb])
                    pyb, kpyb = ps_next()
                    P.add("pe", f_mm(pyb[:, :], [(wbo[:, k, q], bT[:, k, sl(s)]) for k in range(4)]),
                          reads=[kwbo] + [("b", k, s) for k in range(4)], writes=[kpyb])
                    t1, kt1 = tmp_next()
                    t2, kt2 = tmp_next()
                    P.add("act", f_act(t1[:, :], pga[:, :], AF.Sigmoid, bias=cv(l, 20 + dc)), reads=[kpga, ("colv",)], writes=[kt1])
                    P.add("dve", f_tt(t1[:, :], t1[:, :], pya[:, :], ALU.mult), reads=[kt1, kpya], writes=[kt1])
                    P.add("act", f_act(t2[:, :], pgb[:, :], AF.Sigmoid, bias=cv(l, 28 + dc)), reads=[kpgb, ("colv",)], writes=[kt2])
                    P.add("dve", f_tt(t2[:, :], t2[:, :], pyb[:, :], ALU.mult), reads=[kt2, kpyb], writes=[kt2])
                    P.add("dve", f_tt(mT[:, dc, sl(s)], t1[:, :], t2[:, :], ALU.add), reads=[kt1, kt2], writes=[("m", dc, s)])
        wo = [load_w(w_o[l, :, hf * 512:(hf + 1) * 512], 8, 512) for hf in range(2)]
        load_ln(ln1_g, ln1_b, l)
        kt = (l == 1)

        def wo_tiles(tts):
            for tt in tts:
                for hf in range(2):
                    pz, kpz = ps_next()
                    P.add("pe", f_mm(pz[:, :], [(mT[:, k, tt * 128:(tt + 1) * 128], wo[hf][0][:, k, :]) for k in range(8)]),
                          reads=[wo[hf][1]] + [("m", k, tt // 4) for k in range(8)], writes=[kpz])
                    xs = xres[:, tt, hf * 512:(hf + 1) * 512]
                    P.add("dve", f_stt(xs, xs, ALPHA, pz[:, :], ALU.mult, ALU.add),
                          reads=[kpz, ("xres", tt, hf)], writes=[("xres", tt, hf)])
        wo_tiles(range(8) if kt else range(4))
        ln_compute([0, 1, 2, 3], kt)
        if not kt:
            wo_tiles(range(4, 8))
        ln_xpose([0, 1, 2, 3], kt)
        return {"c45": lambda: ln_compute([4, 5], kt), "c67": lambda: ln_compute([6, 7], kt),
                "x45": lambda: ln_xpose([4, 5], kt), "x67": lambda: ln_xpose([6, 7], kt)}

    router_bank = {}

    def router_logits(half):
        pl, kpl = ps_next()
        tts = range(half * 4, half * 4 + 4)

        def fn(pe):
            inst = None
            for tt in tts:
                for k in range(8):
                    inst = pe.matmul(pl[:, tt * NE:(tt + 1) * NE], xT[:, k, tt * 128:(tt + 1) * 128], wr[:, k, :],
                                     start=(k == 0), stop=False)
                inst = pe.matmul(pl[:, tt * NE:(tt + 1) * NE], ones_row[0:1, :], brow_r[0:1, :], start=False, stop=True)
            return inst
        P.add("pe", fn, reads=[("xT", tt) for tt in tts] + [("wr",), ("brow_r",), ("ones_row",)], writes=[kpl])
        cs = slice(half * 4 * NE, (half * 4 + 4) * NE)
        P.add("dve", f_copy(lgt[:, cs], pl[:, cs]), reads=[kpl], writes=[("lgt", half)])

    def router_tt(tt):
        pl, kpl = lgt[:, tt * NE:(tt + 1) * NE], ("lgt", tt // 4)
        s = sst[rr("sst", 4)]
        ks = ("sst", id(s))
        L, m1, mk1, L2, m2, mk2, dd, w1, w2 = (s[:, 16:24], s[:, 12:13], s[:, 24:32], s[:, 32:40], s[:, 13:14],
                                                 s[:, 40:48], s[:, 14:15], s[:, 15:16], s[:, 0:1])
        g = gate[:, tt, :]
        seq = [
            f_copy(L, pl),
            lambda e: e.tensor_reduce(out=m1, in_=L, axis=AX.X, op=ALU.max),
            f_ts(mk1, L, m1, None, ALU.is_equal),
            f_stt(L2, mk1, -1e30, L, ALU.mult, ALU.add),
            lambda e: e.tensor_reduce(out=m2, in_=L2, axis=AX.X, op=ALU.max),
            f_ts(mk2, L2, m2, None, ALU.is_equal),
            f_tt(dd, m1, m2, ALU.subtract),
        ]
        for i, fn in enumerate(seq):
            P.add("dve", fn, reads=[ks] + ([kpl] if i == 0 else []), writes=[ks])
        P.add("act", f_act(w1, dd, AF.Sigmoid), reads=[ks], writes=[ks])
        P.add("act", f_act(w2, dd, AF.Sigmoid, scale=-1.0), reads=[ks], writes=[ks])
        P.add("dve", f_tt(mskf[:, tt, :], mk1, mk2, ALU.add), reads=[ks], writes=[("mskf", tt)])
        P.add("dve", f_copy(msk[:, tt, :], mskf[:, tt, :]), reads=[("mskf", tt)], writes=[("msk", tt)])
        P.add("dve", f_ts(g, mk1, w1, None, ALU.mult), reads=[ks], writes=[("gate", tt)])
        P.add("dve", f_stt(g, mk2, w2, g, ALU.mult, ALU.add), reads=[ks, ("gate", tt)], writes=[("gate", tt)])

    def ranks():
        pr, kpr = ps_next()

        def fn(pe):
            inst = None
            for tt in range(NT):
                pairs = [(triu[:, :], msk[:, tt, :])] + [(ones_b[:, :], msk[:, t2, :]) for t2 in range(tt)]
                for i, (l_, r_) in enumerate(pairs):
                    inst = pe.matmul(pr[:, tt * NE:(tt + 1) * NE], l_, r_, start=(i == 0), stop=(i == len(pairs) - 1))
            return inst
        P.add("pe", fn, reads=[("triu",), ("ones_b",)] + [("msk", t2) for t2 in range(NT)], writes=[kpr])
        for tt in range(NT):
            P.add("dve", f_stt(rk[:, tt, :], pr[:, tt * NE:(tt + 1) * NE], 1.0, mskf[:, tt, :], ALU.add, ALU.mult),
                  reads=[kpr, ("mskf", tt)], writes=[("rk", tt)])
            P.add("dve", f_ts(rk[:, tt, :], rk[:, tt, :], -1.0, None, ALU.add), reads=[("rk", tt)], writes=[("rk", tt)])

    def moe_sparse():
        def S_ap(e, tt):
            buf = cacc_b if e % 2 == 0 else sqt_b
            return buf[:, tt // 2, (tt % 2) * 512:(tt % 2) * 512 + CAP]

        def S_key(e, tt):
            return ("cacc" if e % 2 == 0 else "sqt", tt // 2)

        def SeT_ap(jc):
            buf = lng_b if jc < 2 else lnb_b
            return buf[:, (jc % 2) * 1024:(jc % 2 + 1) * 1024]

        def SeT_key(jc):
            return ("lng",) if jc < 2 else ("lnb",)

        def xg_ap(k):
            return a2T[:, k // 2, (k % 2) * 512:(k % 2) * 512 + CAP]

        def xg_key(k):
            return ("a2", k // 2, k % 2)

        S_keys = lambda e: [S_key(e, tt) for tt in range(0, NT, 2)]

        def build_S(e):
            for tt in range(NT):
                P.add("dve", f_ts(S_ap(e, tt), iota_t[:, 0:CAP], rk[:, tt, e:e + 1], None, ALU.is_equal),
                      reads=[("rk", tt), ("iota",)], writes=[S_key(e, tt)])

        def gather(e):
            for k in range(8):
                pgt, kp = ps_next()
                P.add("pe", f_mm(pgt[:, 0:CAP], [(mT[:, tt, k * 128:(k + 1) * 128], S_ap(e, tt)) for tt in range(NT)]),
                      reads=[("m", tt, k // 4) for tt in range(NT)] + S_keys(e), writes=[kp])
                P.add("act", f_act(xg_ap(k), pgt[:, 0:CAP], AF.Copy), reads=[kp], writes=[xg_key(k)])

        def transp(e):
            for jc in range(NJ):
                pt, kpt = pst_next()
                P.add("pe", f_tr([(pt[:, tt * 128:(tt + 1) * 128], S_ap(e, tt)[:, jc * 128:(jc + 1) * 128])
                                  for tt in range(NT)], ident[:, :]),
                      reads=S_keys(e) + [("ident",)], writes=[kpt])
                P.add("act", f_act(SeT_ap(jc), pt[:, :], AF.Copy), reads=[kpt], writes=[SeT_key(jc)])

        def GU(wgv, kwg, wuv, kwu):
            hi = rr("h", 2)
            hbuf = hT[hi]
            for c in range(4):
                q = slice(c * 128, (c + 1) * 128)
                pg, kpg = ps_next()
                P.add("pe", f_mm(pg[:, 0:CAP], [(wgv[:, k, q], xg_ap(k)) for k in range(8)]),
                      reads=[kwg] + [xg_key(k) for k in range(8)], writes=[kpg])
                pu, kpu = ps_next()
                P.add("pe", f_mm(pu[:, 0:CAP], [(wuv[:, k, q], xg_ap(k)) for k in range(8)]),
                      reads=[kwu] + [xg_key(k) for k in range(8)], writes=[kpu])
                tm, ktm = tmp_next()
                P.add("act", f_act(tm[:, 0:CAP], pg[:, 0:CAP], AF.Silu), reads=[kpg], writes=[ktm])
                P.add("dve", f_tt(hbuf[:, c, 0:CAP], tm[:, 0:CAP], pu[:, 0:CAP], ALU.mult), reads=[ktm, kpu], writes=[("h", hi, c)])
            return hbuf, ("h", hi)

        def Dn(hbuf, hkey, wdv, kwd, first, last):
            for j4 in range(NJ):
                for hf in range(2):
                    hs = slice(hf * 512, (hf + 1) * 512)
                    pd, kpd = ps_next()
                    P.add("pe", f_mm(pd[:, :], [(hbuf[:, c, j4 * 128:(j4 + 1) * 128], wdv[:, c, hs]) for c in range(4)]),
                          reads=[kwd] + [hkey + (c,) for c in range(4)], writes=[kpd])
                    ya = cin[:, j4, HALO + hf * 512:HALO + (hf + 1) * 512]
                    yk = ("cin", j4, hf)
                    if first:
                        P.add("act", f_act(ya, pd[:, :], AF.Copy), reads=[kpd], writes=[yk])
                    elif last:
                        P.add("dve", f_tt(bT[:, j4, hs], ya, pd[:, :], ALU.add), reads=[kpd, yk], writes=[("b", j4, hf)])
                    else:
                        P.add("dve", f_tt(ya, ya, pd[:, :], ALU.add), reads=[kpd, yk], writes=[yk])

        def scatter(e):
            for tt in range(NT):
                for hf in range(2):
                    hs = slice(hf * 512, (hf + 1) * 512)
                    psc, kps = ps_next()
                    P.add("pe", f_mm(psc[:, :], [(SeT_ap(jc)[:, tt * 128:(tt + 1) * 128], bT[:, jc, hs]) for jc in range(NJ)]),
                          reads=[("lng",), ("lnb",)] + [("b", jc, hf) for jc in range(NJ)], writes=[kps])
                    xs = xres[:, tt, hs]
                    P.add("dve", f_stt(xs, psc[:, :], gate[:, tt, e:e + 1], xs, ALU.mult, ALU.add),
                          reads=[kps, ("xres", tt, hf), ("gate", tt)], writes=[("xres", tt, hf)])

        NFB = DEXP // 512
        build_S(0)
        gather(0)
        transp(0)
        for e in range(NE):
            prev = None
            for fb in range(NFB):
                cs = slice(fb * 512, (fb + 1) * 512)
                wgv, kwg = load_w_scr(sg[e][:, cs], 8, 512, e, sg)
                wuv, kwu = load_w_scr(su[e][:, cs], 8, 512, e, su)
                wdv, kwd = load_w_scr(sd[e][cs, :], 4, 1024, e, sd)
                hbuf, hkey = GU(wgv, kwg, wuv, kwu)
                if fb == 0 and e > 0:
                    scatter(e - 1)
                    transp(e)
                if prev is not None:
                    Dn(*prev, first=(fb == 1), last=False)
                if fb == 1 and e < NE - 1:
                    build_S(e + 1)
                prev = (hbuf, hkey, wdv, kwd)
            if e < NE - 1:
                gather(e + 1)
            Dn(*prev, first=False, last=True)
        scatter(NE - 1)

    def ple(l, b, pre=None, post=None):
        t0 = b * TB
        wpg = [load_w(w_ple_gate[l, :, hf * 512:(hf + 1) * 512], 8, 512) for hf in range(2)]
        wpp, kwpp = load_w(w_ple_proj[l, :, :], 2, 1024)
        P.add("pool", f_dma(brow_pg[:, :], b_ple_gate[0:1, l * D:(l + 1) * D]), writes=[("brow_pg",)], dma="bpg")
        for tt in range(NT):
            i = rr("ptok", 2)
            P.add("pool", f_dma(ptok[i][:, :], p_in[l, t0 + tt * 128:t0 + (tt + 1) * 128, :]),
                  writes=[("ptok", i)], dma=f"ptok{i}")
            transposes_to(ptok[i], ("ptok", i), 2, pT, ("pT", tt), tt)
        for tt in range(NT):
            for f in (pre or {}).get(tt, ()):
                f()
            tsl = slice(tt * 128, (tt + 1) * 128)
            for hf in range(2):
                hs = slice(hf * 512, (hf + 1) * 512)
                pg, kpg = ps_next()
                pairs = [(xT[:, k, tsl], wpg[hf][0][:, k, :]) for k in range(8)]
                pairs.append((ones_row[0:1, :], brow_pg[0:1, hf * 512:(hf + 1) * 512]))
                P.add("pe", f_mm(pg[:, :], pairs), reads=[("xT", tt), wpg[hf][1], ("brow_pg",), ("ones_row",)], writes=[kpg])
                pp, kpp = ps_next()
                P.add("pe", f_mm(pp[:, :], [(pT[:, q, tsl], wpp[:, q, hs]) for q in range(2)]),
                      reads=[("pT", tt), kwpp], writes=[kpp])
                tm, ktm = tmp_next()
                P.add("act", f_act(tm[:, :], pg[:, :], AF.Sigmoid), reads=[kpg], writes=[ktm])
                P.add("dve", f_tt(tm[:, :], tm[:, :], pp[:, :], ALU.mult), reads=[ktm, kpp], writes=[ktm])
                xs = xres[:, tt, hs]
                P.add("dve", f_stt(xs, xs, ALPHA, tm[:, :], ALU.mult, ALU.add),
                      reads=[ktm, ("xres", tt, hf)], writes=[("xres", tt, hf)])
            for f in (post or {}).get(tt, ()):
                f()

    def ffn(experts, before_last=None):
        items = []
        for (Wg, Wu, Wd, F, gcol) in experts:
            nchunks = F // 128
            f0 = 0
            while f0 < nchunks:
                nf = min(4, nchunks - f0)
                items.append((Wg, Wu, Wd, f0, nf, gcol))
                f0 += nf
        pend = []

        def emit_down(dn):
            (hbuf, hkey, wd, kwd, nf, gcol, s) = dn
            for t4 in range(4):
                tt = s * 4 + t4
                for hf in range(2):
                    pd, kpd = ps_next()
                    P.add("pe", f_mm(pd[:, :], [(hbuf[:, c, t4 * 128:(t4 + 1) * 128], wd[:, c, hf * 512:(hf + 1) * 512])
                                                 for c in range(nf)]),
                          reads=[kwd] + [hkey + (c,) for c in range(nf)], writes=[kpd])
                    xs = xres[:, tt, hf * 512:(hf + 1) * 512]
                    if gcol is None:
                        P.add("dve", f_tt(xs, xs, pd[:, :], ALU.add), reads=[kpd, ("xres", tt, hf)], writes=[("xres", tt, hf)])
                    else:
                        P.add("dve", f_stt(xs, pd[:, :], gate[:, tt, gcol:gcol + 1], xs, ALU.mult, ALU.add),
                              reads=[kpd, ("xres", tt, hf), ("gate", tt)], writes=[("xres", tt, hf)])

        for (Wg, Wu, Wd, f0, nf, gcol) in items:
            cs = slice(f0 * 128, (f0 + nf) * 128)
            wgv, kwg = load_w(Wg[:, cs], 8, nf * 128)
            wuv, kwu = load_w(Wu[:, cs], 8, nf * 128)
            wdv, kwd = load_w(Wd[cs, :], nf, 1024)
            for s in range(NSUB):
                hi = rr("h", 2)
                hbuf = hT[hi]
                hkey = ("h", hi)
                for c in range(nf):
                    q = slice(c * 128, (c + 1) * 128)
                    pg, kpg = ps_next()
                    P.add("pe", f_mm(pg[:, :], [(wgv[:, k, q], xT[:, k, s * SUB:(s + 1) * SUB]) for k in range(8)]),
                          reads=[kwg] + xT_keys(s), writes=[kpg])
                    pu, kpu = ps_next()
                    P.add("pe", f_mm(pu[:, :], [(wuv[:, k, q], xT[:, k, s * SUB:(s + 1) * SUB]) for k in range(8)]),
                          reads=[kwu] + xT_keys(s), writes=[kpu])
                    tm, ktm = tmp_next()
                    P.add("act", f_act(tm[:, :], pg[:, :], AF.Silu), reads=[kpg], writes=[ktm])
                    P.add("dve", f_tt(hbuf[:, c, :], tm[:, :], pu[:, :], ALU.mult), reads=[ktm, kpu], writes=[hkey + (c,)])
                pend.append((hbuf, hkey, wdv, kwd, nf, gcol, s))
                if len(pend) > 1:
                    emit_down(pend.pop(0))
        while pend:
            if len(pend) == 1 and before_last is not None:
                before_last()
            emit_down(pend.pop(0))

    last_l = layers[-1]
    first_l = layers[0]

    stage_bufs = [(xtok[i][:, :], [("xtok", i)], f"xtok{i}") for i in range(2)] + \
                 [(pT[:, q, :], [("pT", t) for t in range(NT)], f"ppT{q}") for q in range(2)] + \
                 [(tmps[i][:, :].bitcast(BF16), [("tmp", i)], f"ptmp{i}") for i in range(NTMP)]

    def prologue(b):
        t0 = b * TB
        loaded = []
        for tt in range(NT):
            buf, key, sem = stage_bufs[tt % len(stage_bufs)]
            P.add("pool", f_dma(buf, x[t0 + tt * 128:t0 + (tt + 1) * 128, :]), writes=key, dma=sem)
            loaded.append((buf, key))
        for tt, (bb, kk) in enumerate(loaded):
            pt, kpt = pst_next()
            P.add("pe", f_tr([(pt[:, k * 128:(k + 1) * 128], bb[:, k * 128:(k + 1) * 128]) for k in range(8)], ident[:, :]),
                  reads=kk + [("ident",)], writes=[kpt])
            P.add("act", f_act(xT[:, :, tt * 128:(tt + 1) * 128], pt[:, :].rearrange("p (k t) -> p k t", k=8), AF.Copy),
                  reads=[kpt], writes=[("xT", tt)])

    def load_x(b):
        t0 = b * TB
        P.add("sp", f_dma(xres[:, :, :], x[t0:t0 + TB, :].rearrange("(t p) d -> p t d", p=128)),
              writes=[("xres", tt, hf) for tt in range(NT) for hf in range(2)], dma="xin")

    def store_out(b):
        t0 = b * TB
        P.add("sp", f_dma(out[t0:t0 + TB, :].rearrange("(t p) d -> p t d", p=128), xres[:, :, :]),
              reads=[("xres", tt, hf) for tt in range(NT) for hf in range(2)], writes=[("out", b)], dma="out")

    prologue(0)
    mixer_A(first_l, 0)
    load_x(0)
    carry = None
    for b in range(n_blk):
        P.epoch = b
        for l in layers:
            if l == first_l:
                ln1_rest = mixer_rest(l, b, postB=carry)
                carry = None
            else:
                ln1_rest = mixer_rest(l, b)
            r = ln1_rest
            pre = {2: [r["c45"]], 3: [r["c67"]], 4: [r["x45"]], 6: [r["x67"]]}
            if l == 1:
                pre[4] = [r["x45"], partial(router_logits, 0)]
                pre[6] = [r["x67"], partial(router_logits, 1)]
                post = {4 + i: [partial(router_tt, 2 * i), partial(router_tt, 2 * i + 1)] for i in range(4)}
                ple(l, b, pre=pre, post=post)
                ranks()
                moe_sparse()
                load_ln(ln2_g, ln2_b, l)
            else:
                ple(l, b, pre=pre)
                load_ln(ln2_g, ln2_b, l)
                ffn([(w_ff_gate[0], w_ff_up[0], w_ff_down[0], DFF, None)],
                    before_last=(lambda: ln_compute([0, 1, 2, 3])) if l != last_l else None)
            if l != last_l:
                nl = layers[layers.index(l) + 1]
                ln_xpose([0, 1, 2, 3])
                mixer_A(nl, b, post={(0, 0): [lambda: ln_compute([4, 5])], (0, 1): [lambda: ln_compute([6, 7])],
                                     (0, 3): [lambda: ln_xpose([4, 5, 6, 7])]})
            elif b + 1 < n_blk:
                prologue(b + 1)
                mixer_A(first_l, b + 1)
                carry = {0: [lambda: ln_group([0, 1, 2, 3], False)],
                         1: [lambda: ln_group([4, 5, 6, 7], False), partial(store_out, b), partial(load_x, b + 1)]}
            else:
                layer_norm_all(False)
                store_out(b)
    P.add("sp", None, reads=[("out", b) for b in range(n_blk)])

    P.emit(nc, es)
    es.close()
    return nc


def _colvecs(inp):
    cols = np.zeros((128, 2 * NCOLV), np.float32)
    for l in range(2):
        base = l * NCOLV
        cols[:, base:base + 36] = np.asarray(inp["b_in"][l]).reshape(36, 128).T
        cw = np.asarray(inp["conf_conv_w"][l]).reshape(KCONF, 4, 128)
        cols[:, base + 36:base + 160] = cw.transpose(2, 1, 0).reshape(128, 4 * KCONF)
        cols[:, base + 160:base + 164] = np.asarray(inp["conf_conv_b"][l]).reshape(4, 128).T
        cols[:, base + 164:base + 168] = np.asarray(inp["conf_ln_g"][l]).reshape(4, 128).T
        cols[:, base + 168:base + 172] = np.asarray(inp["conf_ln_b"][l]).reshape(4, 128).T
        sw = np.asarray(inp["sc_conv_w"][l]).reshape(KSC, 4, 128)
        cols[:, base + 172:base + 184] = sw.transpose(2, 1, 0).reshape(128, 4 * KSC)
    return cols


_NC_CACHE = {}


def _get_nc(n_blk, layers):
    key = (n_blk, tuple(layers))
    if key not in _NC_CACHE:
        _NC_CACHE[key] = build_nc(n_blk, layers)
    return _NC_CACHE[key]


def make_in_maps(inp, cores):
    f = lambda a: np.ascontiguousarray(np.asarray(a, dtype=np.float32))
    shared = {k: f(inp[k]) for k in ("w_in", "w_conf_out", "w_sc_out", "w_o", "w_ff_gate", "w_ff_up", "w_ff_down",
                                     "w_router", "b_router", "we_gate", "we_up", "we_down", "w_ple_gate",
                                     "w_ple_proj", "ln1_g", "ln1_b", "ln2_g", "ln2_b")}
    shared["b_ple_gate"] = f(inp["b_ple_gate"]).reshape(1, 2 * D)
    shared["colv"] = _colvecs(inp)
    shared["ident"] = np.eye(128, dtype=np.float32)
    shared["iota"] = np.ascontiguousarray(np.broadcast_to(np.arange(512, dtype=np.float32), (128, 512)))
    shared["triu"] = np.triu(np.ones((128, 128), np.float32), k=1)
    xs = np.asarray(inp["x"], dtype=np.float32)
    ps_ = np.asarray(inp["p"], dtype=np.float32)
    maps = []
    for c in cores:
        m = dict(shared)
        m["x"] = np.ascontiguousarray(xs[c])
        m["p"] = np.ascontiguousarray(ps_[:, c])
        maps.append(m)
    return maps


def kernel(**inputs):
    nc = _get_nc(NBLK, (0, 1))
    in_maps = make_in_maps(inputs, list(range(NCORES)))
    res = run_bass_kernel_spmd(nc, in_maps, core_ids=list(range(NCORES)))
    return np.stack([np.asarray(r["out"], dtype=np.float32) for r in res.results], axis=0)
```

```python
import numpy as np
from contextlib import ExitStack
from functools import partial
import concourse.bass as bass
import concourse.mybir as mybir
from concourse.bass_utils import run_bass_kernel_spmd

F32 = mybir.dt.float32
BF16 = mybir.dt.bfloat16
AF = mybir.ActivationFunctionType
ALU = mybir.AluOpType
AX = mybir.AxisListType

D = 1024
SEQ = 4096
NCORES = 8
TB = 1024
NBLK = SEQ // TB
SUB = 512
NSUB = TB // SUB
NT = TB // 128
DPLE = 256
KCONF = 31
KSC = 3
DFF = 2816
NE = 8
DEXP = 3584
INC = 4608
ALPHA = float(4 ** 0.25)
EPS = 1e-5
HALO = 32
NCOLV = 184
NSLOT = 6
NPS = 6
NTMP = 5
CAP = 384
NJ = CAP // 128


class Op:
    __slots__ = ("eng", "fn", "dma_sem", "dma_cnt", "idx", "epoch", "waits", "signal", "sigcnt")


class Prog:
    ENGS = ("pe", "act", "dve", "pool", "sp")

    def __init__(self):
        self.ops = {e: [] for e in self.ENGS}
        self.last_w = {}
        self.readers = {}
        self.waited = {e: {} for e in self.ENGS}
        self.waited_dma = {e: {} for e in self.ENGS}
        self.dma_counts = {}
        self.epoch = 0

    def add(self, eng, fn, reads=(), writes=(), dma=None):
        op = Op()
        op.eng = eng
        op.fn = fn
        op.dma_sem = dma
        op.dma_cnt = 0
        op.epoch = self.epoch
        op.waits = []
        op.signal = False
        op.sigcnt = 0
        op.idx = len(self.ops[eng])
        if dma is not None:
            self.dma_counts[dma] = self.dma_counts.get(dma, 0) + 16
            op.dma_cnt = self.dma_counts[dma]
        deps = []
        for k in reads:
            w = self.last_w.get(k)
            if w is not None:
                deps.append(w)
        for k in writes:
            w = self.last_w.get(k)
            if w is not None:
                deps.append(w)
            deps.extend(self.readers.get(k, ()))
        for d in deps:
            if d.dma_sem is not None:
                cur = self.waited_dma[eng].get(d.dma_sem, 0)
                if d.dma_cnt > cur:
                    self.waited_dma[eng][d.dma_sem] = d.dma_cnt
                    op.waits.append(("dma", d.dma_sem, d.dma_cnt))
            else:
                if d.eng == eng and eng == "pe":
                    continue
                cur = self.waited[eng].get(d.eng, -1)
                if d.idx > cur:
                    self.waited[eng][d.eng] = d.idx
                    d.signal = True
                    op.waits.append(("eng", d))
        for k in writes:
            self.last_w[k] = op
            self.readers[k] = []
        for k in reads:
            if k not in writes:
                self.readers.setdefault(k, []).append(op)
        self.ops[eng].append(op)
        return op

    def finalize(self):
        self.max_epoch = 0
        for e in self.ENGS:
            cnt = {}
            for op in self.ops[e]:
                if op.signal:
                    cnt[op.epoch] = cnt.get(op.epoch, 0) + 1
                    op.sigcnt = cnt[op.epoch]
                self.max_epoch = max(self.max_epoch, op.epoch)

    def emit(self, nc, es):
        self.finalize()
        nep = self.max_epoch + 1
        esem = {e: [es.enter_context(nc.semaphore(f"s_{e}_{i}")) for i in range(nep)] for e in self.ENGS}
        dsem = {n: es.enter_context(nc.semaphore(f"d_{n}")) for n in self.dma_counts}
        block = es.enter_context(nc.Block())

        def run(e, h):
            for op in self.ops[e]:
                for w in op.waits:
                    if w[0] == "dma":
                        h.wait_ge(dsem[w[1]], w[2])
                    else:
                        d = w[1]
                        h.wait_ge(esem[d.eng][d.epoch], d.sigcnt)
                if op.fn is None:
                    continue
                inst = op.fn(h)
                if op.dma_sem is not None:
                    inst.then_inc(dsem[op.dma_sem], 16)
                elif op.signal:
                    inst.then_inc(esem[e][op.epoch], 1)

        block.tensor(partial(run, "pe"))
        block.scalar(partial(run, "act"))
        block.vector(partial(run, "dve"))
        block.gpsimd(partial(run, "pool"))
        block.sync(partial(run, "sp"))


def f_mm(out, pairs):
    def fn(pe):
        n = len(pairs)
        inst = None
        for i, (l, r) in enumerate(pairs):
            inst = pe.matmul(out, l, r, start=(i == 0), stop=(i == n - 1))
        return inst
    return fn


def f_tr(outs_ins, ident):
    def fn(pe):
        inst = None
        for o, i in outs_ins:
            inst = pe.transpose(o, i, ident)
        return inst
    return fn


def f_act(out, in_, func, bias=None, scale=None):
    def fn(e):
        kw = {}
        if bias is not None:
            kw["bias"] = bias
        if scale is not None:
            kw["scale"] = scale
        return e.activation(out=out, in_=in_, func=func, **kw)
    return fn


def f_stt(out, in0, scalar, in1, op0, op1):
    return lambda e: e.scalar_tensor_tensor(out=out, in0=in0, scalar=scalar, in1=in1, op0=op0, op1=op1)


def f_tt(out, in0, in1, op):
    return lambda e: e.tensor_tensor(out=out, in0=in0, in1=in1, op=op)


def f_ts(out, in0, s1, s2, op0, op1=None):
    if op1 is None:
        return lambda e: e.tensor_scalar(out=out, in0=in0, scalar1=s1, scalar2=None, op0=op0)
    return lambda e: e.tensor_scalar(out=out, in0=in0, scalar1=s1, scalar2=s2, op0=op0, op1=op1)


def f_recip(ap):
    return lambda e: e.reciprocal(out=ap, in_=ap)


def f_copy(out, in_):
    return lambda e: e.tensor_copy(out=out, in_=in_)


def f_memset(ap, v):
    return lambda e: e.memset(ap, v)


def f_dma(out, in_):
    return lambda e: e.dma_start(out=out, in_=in_)


def build_nc(n_blk=NBLK, layers=(0, 1)):
    nc = bass.Bass("TRN2", target_bir_lowering=False)

    def din(name, shape):
        return nc.dram_tensor(name, list(shape), F32, kind="ExternalInput").ap()

    x = din("x", [SEQ, D])
    p_in = din("p", [2, SEQ, DPLE])
    w_in = din("w_in", [2, D, INC])
    w_conf_out = din("w_conf_out", [2, 512, D])
    w_sc_out = din("w_sc_out", [2, 512, D])
    w_o = din("w_o", [2, D, D])
    w_ff_gate = din("w_ff_gate", [1, D, DFF])
    w_ff_up = din("w_ff_up", [1, D, DFF])
    w_ff_down = din("w_ff_down", [1, DFF, D])
    w_router = din("w_router", [1, D, NE])
    b_router = din("b_router", [1, NE])
    we_gate = din("we_gate", [1, NE, D, DEXP])
    we_up = din("we_up", [1, NE, D, DEXP])
    we_down = din("we_down", [1, NE, DEXP, D])
    w_ple_gate = din("w_ple_gate", [2, D, D])
    b_ple_gate = din("b_ple_gate", [1, 2 * D])
    w_ple_proj = din("w_ple_proj", [2, DPLE, D])
    ln1_g = din("ln1_g", [2, D])
    ln1_b = din("ln1_b", [2, D])
    ln2_g = din("ln2_g", [2, D])
    ln2_b = din("ln2_b", [2, D])
    colv_d = din("colv", [128, 2 * NCOLV])
    ident_d = din("ident", [128, 128])
    iota_d = din("iota", [128, 512])
    triu_d = din("triu", [128, 128])
    out = nc.dram_tensor("out", [SEQ, D], F32, kind="ExternalOutput").ap()
    sg = nc.dram_tensor("scr_g", [NE, D, DEXP], BF16, kind="Internal").ap()
    su = nc.dram_tensor("scr_u", [NE, D, DEXP], BF16, kind="Internal").ap()
    sd = nc.dram_tensor("scr_d", [NE, DEXP, D], BF16, kind="Internal").ap()

    es = ExitStack()
    P = Prog()

    def sb(name, shape, dt):
        return es.enter_context(nc.sbuf_tensor(name, list(shape), dt))

    xres = sb("xres", [128, NT, D], F32)
    xT = sb("xT", [128, 8, TB], BF16)
    xtok = [sb(f"xtok{i}", [128, D], BF16) for i in range(2)]
    cin = sb("cin", [128, 4, HALO + TB], F32)
    a2T = sb("a2T", [128, 4, TB], BF16)
    bT = sb("bT", [128, 4, TB], BF16)
    mT = sb("mT", [128, 8, TB], BF16)
    cacc = sb("cacc", [128, 4, SUB], F32)
    sqt = sb("sqt", [128, 4, SUB], F32)
    tmps = [sb(f"tmp{i}", [128, SUB], F32) for i in range(NTMP)]
    hT = [sb(f"hT{i}", [128, 4, SUB], BF16) for i in range(2)]
    wsl = [sb(f"wsl{i}", [128, 4096], BF16) for i in range(NSLOT)]
    lng = sb("lng", [128, D], F32)
    lnb = sb("lnb", [128, D], F32)
    ptok = [sb(f"ptok{i}", [128, DPLE], BF16) for i in range(2)]
    pT = sb("pT", [128, 2, TB], BF16)
    colv = sb("colv_s", [128, 2 * NCOLV], F32)
    halo_a = [sb(f"halo_a{l}", [128, 4, HALO], F32) for l in range(2)]
    halo_c = [sb(f"halo_c{l}", [128, 4, HALO], F32) for l in range(2)]
    ident = sb("ident_s", [128, 128], BF16)
    ones_f = sb("ones_f", [128, 128], F32)
    ones_row = sb("ones_row", [1, 128], BF16)
    wr = sb("wr", [128, 8, NE], BF16)
    brow_r = sb("brow_r", [1, NE], BF16)
    brow_pg = sb("brow_pg", [1, D], BF16)
    gate = sb("gate", [128, NT, NE], F32)
    sst = [sb(f"sst{i}", [128, 48], F32) for i in range(4)]
    eps_t = sb("eps_t", [128, 1], F32)
    lnst = sb("lnst", [128, NT, 16], F32)
    lgt = sb("lgt", [128, NT * NE], F32)
    iota_t = sb("iota_t", [128, 512], F32)
    triu = sb("triu_s", [128, 128], BF16)
    ones_b = sb("ones_b", [128, 128], BF16)
    msk = sb("msk", [128, NT, NE], BF16)
    mskf = sb("mskf", [128, NT, NE], F32)
    rk = sb("rk", [128, NT, NE], F32)
    cacc_b = cacc[:, :, :].bitcast(BF16)
    cin_b = cin[:, :, :].bitcast(BF16)
    halo_ab = [h[:, :, :].bitcast(BF16) for h in halo_a]
    dgv = [h[:, :, :].rearrange("p a (b q) -> p (a b) q", q=128) for h in hT]
    sqt_b = sqt[:, :, :].bitcast(BF16)
    lng_b = lng[:, :].bitcast(BF16)
    lnb_b = lnb[:, :].bitcast(BF16)

    ps = [es.enter_context(nc.psum_tensor(f"ps{i}", [128, SUB], F32)) for i in range(NPS)]
    pst = [es.enter_context(nc.psum_tensor(f"pst{i}", [128, 1024], BF16)) for i in range(2)]

    cnt = {"ps": 0, "pst": 0, "tmp": 0, "w": 0, "sst": 0, "xtok": 0, "ptok": 0, "h": 0}

    def rr(name, n):
        i = cnt[name] % n
        cnt[name] += 1
        return i

    def ps_next():
        i = rr("ps", NPS)
        return ps[i], ("ps", i)

    def pst_next():
        i = rr("pst", 2)
        return pst[i], ("pst", i)

    def tmp_next():
        i = rr("tmp", NTMP)
        return tmps[i], ("tmp", i)

    conv_units = []
    if 1 in layers:
        for e in range(NE):
            for (dst, srcw, rows) in ((sg, we_gate, D), (su, we_up, D), (sd, we_down, DEXP)):
                for j in range(4):
                    conv_units.append((e, dst, srcw, rows, j))
    conv_state = {"calls": 0}

    def emit_conv_unit():
        if not conv_units:
            return
        e, dst, srcw, rows, j = conv_units.pop(0)
        q = rows // 4
        P.add("pool", f_dma(dst[e, j * q:(j + 1) * q, :], srcw[0, e, j * q:(j + 1) * q, :]),
              writes=[("scr", e, id(dst), j)], dma=f"cv{e}_{rows}_{id(dst) % 9973}")

    def load_w(src_ap, k, c):
        i = rr("w", NSLOT)
        view = wsl[i][:, 0:k * c].rearrange("p (k c) -> p k c", k=k)
        P.add("pool", f_dma(view, src_ap.rearrange("(k p) c -> p k c", p=128)),
              writes=[("w", i)], dma=f"w{i}")
        emit_conv_unit()
        return view, ("w", i)

    def load_w_scr(src_ap, k, c, e, dst):
        while any(u[0] == e for u in conv_units):
            emit_conv_unit()
        i = rr("w", NSLOT)
        view = wsl[i][:, 0:k * c].rearrange("p (k c) -> p k c", k=k)
        P.add("sp", f_dma(view, src_ap.rearrange("(k p) c -> p k c", p=128)),
              reads=[("scr", e, id(dst), j) for j in range(4)], writes=[("w", i)], dma=f"w{i}")
        emit_conv_unit()
        return view, ("w", i)

    P.add("sp", f_dma(colv[:, :], colv_d[:, :]), writes=[("colv",)], dma="c0")
    P.add("pool", f_dma(ident[:, :], ident_d[:, :]), writes=[("ident",)], dma="c1")
    P.add("pool", f_dma(wr[:, :, :], w_router[0].rearrange("(k p) e -> p k e", p=128)),
          writes=[("wr",)], dma="c2")
    P.add("pool", f_dma(brow_r[:, :], b_router[0:1, :]), writes=[("brow_r",)], dma="c3")
    P.add("dve", f_memset(ones_f[:, :], 1.0), writes=[("ones_f",)])
    P.add("dve", f_memset(ones_row[:, :], 1.0), writes=[("ones_row",)])
    P.add("dve", f_memset(eps_t[:, :], EPS), writes=[("eps",)])
    P.add("dve", f_memset(ones_b[:, :], 1.0), writes=[("ones_b",)])
    P.add("sp", f_dma(iota_t[:, :], iota_d[:, :]), writes=[("iota",)], dma="c5")
    P.add("pool", f_dma(triu[:, :], triu_d[:, :]), writes=[("triu",)], dma="c6")

    def cv(l, j):
        return colv[:, l * NCOLV + j:l * NCOLV + j + 1]

    xT_keys = lambda s: [("xT", s * 4 + i) for i in range(4)]

    def transposes_to(src_tok, src_key, n_k, dst, dst_key, tt):
        pt, kpt = pst_next()
        P.add("pe", f_tr([(pt[:, k * 128:(k + 1) * 128], src_tok[:, k * 128:(k + 1) * 128]) for k in range(n_k)],
                         ident[:, :]),
              reads=[src_key, ("ident",)], writes=[kpt])
        P.add("act", f_act(dst[:, 0:n_k, tt * 128:(tt + 1) * 128],
                           pt[:, 0:n_k * 128].rearrange("p (k t) -> p k t", k=n_k), AF.Copy),
              reads=[kpt], writes=[dst_key])

    def layer_norm_all(to_xT, keep_tok=False):
        ln_group(range(NT), to_xT, keep_tok)

    def ln_group(tiles, to_xT, keep_tok=False):
        ln_compute(tiles, keep_tok and to_xT)
        if to_xT:
            ln_xpose(tiles, keep_tok)

    def ln_compute(tiles, keep_tok=False):
        tiles = list(tiles)
        xk = lambda tt: [("xres", tt, 0), ("xres", tt, 1)]
        st = lambda tt, a, b_: lnst[:, tt, a:b_]
        ks = lambda tt, i: ("lnst", tt, i)
        for tt in tiles:
            P.add("dve", partial(lambda e, tt: e.bn_stats(out=lnst[:, tt, 0:6], in_=xres[:, tt, 0:512]), tt=tt),
                  reads=[xk(tt)[0]], writes=[ks(tt, 0)])
            P.add("dve", partial(lambda e, tt: e.bn_stats(out=lnst[:, tt, 6:12], in_=xres[:, tt, 512:1024]), tt=tt),
                  reads=[xk(tt)[1]], writes=[ks(tt, 1)])
            P.add("dve", partial(lambda e, tt: e.bn_aggr(out=lnst[:, tt, 12:14], in_=lnst[:, tt, 0:12]), tt=tt),
                  reads=[ks(tt, 0), ks(tt, 1)], writes=[ks(tt, 2)])
        for tt in tiles:
            P.add("act", f_act(st(tt, 14, 15), st(tt, 13, 14), AF.Sqrt, bias=eps_t[:, 0:1]),
                  reads=[ks(tt, 2), ("eps",)], writes=[ks(tt, 3)])
        for tt in tiles:
            P.add("dve", f_recip(st(tt, 14, 15)), reads=[ks(tt, 3)], writes=[ks(tt, 3)])
        for tt in tiles:
            P.add("dve", f_stt(xres[:, tt, :], xres[:, tt, :], st(tt, 12, 13), lng[:, :], ALU.subtract, ALU.mult),
                  reads=xk(tt) + [ks(tt, 2), ("lng",)], writes=xk(tt))
            P.add("dve", f_stt(xres[:, tt, :], xres[:, tt, :], st(tt, 14, 15), lnb[:, :], ALU.mult, ALU.add),
                  reads=xk(tt) + [ks(tt, 3), ("lnb",)], writes=xk(tt))
        if keep_tok:
            for tt in tiles:
                P.add("act", f_act(mT[:, tt, :], xres[:, tt, :], AF.Copy), reads=xk(tt), writes=[("m", tt, 0), ("m", tt, 1)])

    def ln_xpose(tiles, keep_tok=False):
        xk = lambda tt: [("xres", tt, 0), ("xres", tt, 1)]
        for tt in tiles:
            if keep_tok:
                dk = [("m", tt, 0), ("m", tt, 1)]
                pt, kpt = pst_next()
                P.add("pe", f_tr([(pt[:, k * 128:(k + 1) * 128], mT[:, tt, k * 128:(k + 1) * 128]) for k in range(8)], ident[:, :]),
                      reads=dk + [("ident",)], writes=[kpt])
                P.add("act", f_act(xT[:, :, tt * 128:(tt + 1) * 128], pt[:, :].rearrange("p (k t) -> p k t", k=8), AF.Copy),
                      reads=[kpt], writes=[("xT", tt)])
            else:
                i = rr("xtok", 2)
                P.add("act", f_act(xtok[i][:, :], xres[:, tt, :], AF.Copy), reads=xk(tt), writes=[("xtok", i)])
                transposes_to(xtok[i], ("xtok", i), 8, xT, ("xT", tt), tt)

    def load_ln(g_d, b_d, l):
        P.add("sp", f_dma(lng[:, :], g_d[l:l + 1, :].partition_broadcast(128)), writes=[("lng",)], dma="lng")
        P.add("sp", f_dma(lnb[:, :], b_d[l:l + 1, :].partition_broadcast(128)), writes=[("lnb",)], dma="lnb")

    def set_halo(b, halo):
        hk = [("cin", c, "h") for c in range(4)]
        if b == 0:
            P.add("dve", f_memset(cin[:, :, 0:HALO], 0.0), writes=hk)
        else:
            P.add("dve", f_copy(cin[:, :, 0:HALO], halo[:, :, :]), reads=[("halo", id(halo))], writes=hk)

    def save_halo(halo):
        P.add("dve", f_copy(halo[:, :, :], cin[:, :, TB:TB + HALO]),
              reads=[("cin", c, NSUB - 1) for c in range(4)], writes=[("halo", id(halo))])

    def cin_keys(c, s):
        return [("cin", c, "h" if s == 0 else s - 1), ("cin", c, s)]

    sl = lambda s: slice(s * SUB, (s + 1) * SUB)
    csl = lambda s: slice(HALO + s * SUB, HALO + (s + 1) * SUB)

    def mixer_A(l, b, post=None):
        wv, kwv = load_w(w_in[l, :, 0:512], 8, 512)
        wg, kwg = load_w(w_in[l, :, 512:1024], 8, 512)
        ak = lambda c: [("cin", c, "h"), ("cin", c, 0)]
        if b == 0:
            P.add("dve", f_memset(cin_b[:, :, 0:HALO], 0.0), writes=[k for c in range(4) for k in ak(c)])
        else:
            P.add("dve", f_copy(cin_b[:, :, 0:HALO], halo_ab[l][:, :, 0:HALO]), reads=[("halo", id(halo_a[l]))],
                  writes=[k for c in range(4) for k in ak(c)])
        for s in range(NSUB):
            for c in range(4):
                pv, kpv = ps_next()
                P.add("pe", f_mm(pv[:, :], [(wv[:, k, c * 128:(c + 1) * 128], xT[:, k, sl(s)]) for k in range(8)]),
                      reads=[kwv] + xT_keys(s), writes=[kpv])
                pg, kpg = ps_next()
                P.add("pe", f_mm(pg[:, :], [(wg[:, k, c * 128:(c + 1) * 128], xT[:, k, sl(s)]) for k in range(8)]),
                      reads=[kwg] + xT_keys(s), writes=[kpg])
                tm, ktm = tmp_next()
                P.add("act", f_act(tm[:, :], pg[:, :], AF.Sigmoid, bias=cv(l, 4 + c)), reads=[kpg, ("colv",)], writes=[ktm])
                P.add("dve", f_stt(cin_b[:, c, csl(s)], pv[:, :], cv(l, c), tm[:, :], ALU.add, ALU.mult),
                      reads=[kpv, ktm, ("colv",)], writes=ak(c))
                for f in (post or {}).get((s, c), ()):
                    f()

    def mixer_rest(l, b, postB=None):
        ak = lambda c: [("cin", c, "h"), ("cin", c, 0)]
        def conv_mm(s):
            banks = []
            for c in range(4):
                pc_, kpc_ = ps_next()
                banks.append((pc_, kpc_))
                for hi, (j0, nj) in enumerate(((0, 16), (16, KCONF - 16))):
                    wbase = l * NCOLV + 36 + c * KCONF + j0
                    hk = [("h", hi, q) for q in range(4)]
                    P.add("dve", f_tt(dgv[hi][:, 0:nj, :], ident[:, :].unsqueeze(1).to_broadcast([128, nj, 128]),
                                      colv[:, wbase:wbase + nj].unsqueeze(2).to_broadcast([128, nj, 128]), ALU.mult),
                          reads=[("ident",), ("colv",)], writes=hk)

                    def fn(pe, c=c, s=s, hi=hi, j0=j0, nj=nj, pc_=pc_):
                        inst = None
                        for jj in range(nj):
                            j = j0 + jj
                            inst = pe.matmul(pc_[:, :], dgv[hi][:, jj, :], cin_b[:, c, 2 + j + s * SUB:2 + j + s * SUB + SUB],
                                             start=(j == 0), stop=(j == KCONF - 1))
                        return inst
                    P.add("pe", fn, reads=hk + ak(c), writes=[kpc_])
            return banks

        def conv_evac(banks):
            for c, (pc_, kpc_) in enumerate(banks):
                P.add("act", f_act(cacc[:, c, :], pc_[:, :], AF.Identity, bias=cv(l, 160 + c)),
                      reads=[kpc_, ("colv",)], writes=[("cacc", c)])
            for c in range(4):
                P.add("act", f_act(sqt[:, c, :], cacc[:, c, :], AF.Square), reads=[("cacc", c)], writes=[("sqt", c)])

        def ln_sub(s):
            p1, kp1 = ps_next()
            P.add("pe", f_mm(p1[:, :], [(ones_f[:, :], cacc[:, c, :]) for c in range(4)]),
                  reads=[("ones_f",)] + [("cacc", c) for c in range(4)], writes=[kp1])
            p2, kp2 = ps_next()
            P.add("pe", f_mm(p2[:, :], [(ones_f[:, :], sqt[:, c, :]) for c in range(4)]),
                  reads=[("ones_f",)] + [("sqt", c) for c in range(4)], writes=[kp2])
            tmean, kmean = tmp_next()
            tmsq, kmsq = tmp_next()
            P.add("act", f_act(tmean[:, :], p1[:, :], AF.Copy, scale=1.0 / 512), reads=[kp1], writes=[kmean])
            P.add("act", f_act(tmsq[:, :], p1[:, :], AF.Square, scale=1.0 / 512), reads=[kp1], writes=[kmsq])
            P.add("dve", f_stt(tmsq[:, :], p2[:, :], 1.0 / 512, tmsq[:, :], ALU.mult, ALU.subtract),
                  reads=[kp2, kmsq], writes=[kmsq])
            P.add("act", f_act(tmsq[:, :], tmsq[:, :], AF.Sqrt, bias=eps_t[:, 0:1]), reads=[kmsq, ("eps",)], writes=[kmsq])
            P.add("dve", f_recip(tmsq[:, :]), reads=[kmsq], writes=[kmsq])
            for c in range(4):
                P.add("dve", f_stt(cacc[:, c, :], cacc[:, c, :], 1.0, tmean[:, :], ALU.mult, ALU.subtract),
                      reads=[("cacc", c), kmean], writes=[("cacc", c)])
            for c in range(4):
                P.add("dve", f_stt(cacc[:, c, :], cacc[:, c, :], 1.0, tmsq[:, :], ALU.mult, ALU.mult),
                      reads=[("cacc", c), kmsq], writes=[("cacc", c)])
            for c in range(4):
                P.add("act", f_act(a2T[:, c, sl(s)], cacc[:, c, :], AF.Silu, bias=cv(l, 168 + c), scale=cv(l, 164 + c)),
                      reads=[("cacc", c), ("colv",)], writes=[("a2", c, s)])
            for f in (postB or {}).get(s, ()):
                f()

        banks0 = conv_mm(0)
        conv_evac(banks0)
        banks1 = conv_mm(1)
        P.add("dve", f_copy(halo_ab[l][:, :, 0:HALO], cin_b[:, :, TB:TB + HALO]),
              reads=[k for c in range(4) for k in ak(c)], writes=[("halo", id(halo_a[l]))])
        ln_sub(0)
        conv_evac(banks1)
        wb, kwb = load_w(w_in[l, :, 1024:1536], 8, 512)
        wc, kwc = load_w(w_in[l, :, 1536:2048], 8, 512)
        wh, kwh = load_w(w_in[l, :, 2048:2560], 8, 512)
        set_halo(b, halo_c[l])
        for s in range(NSUB):
            for c in range(4):
                pb, kpb = ps_next()
                P.add("pe", f_mm(pb[:, :], [(wb[:, k, c * 128:(c + 1) * 128], xT[:, k, sl(s)]) for k in range(8)]),
                      reads=[kwb] + xT_keys(s), writes=[kpb])
                ph, kph = ps_next()
                P.add("pe", f_mm(ph[:, :], [(wh[:, k, c * 128:(c + 1) * 128], xT[:, k, sl(s)]) for k in range(8)]),
                      reads=[kwh] + xT_keys(s), writes=[kph])
                pc, kpc = ps_next()
                P.add("pe", f_mm(pc[:, :], [(wc[:, k, c * 128:(c + 1) * 128], xT[:, k, sl(s)]) for k in range(8)]),
                      reads=[kwc] + xT_keys(s), writes=[kpc])
                P.add("act", f_act(bT[:, c, sl(s)], pb[:, :], AF.Identity, bias=cv(l, 8 + c)),
                      reads=[kpb, ("colv",)], writes=[("b", c, s)])
                tm, ktm = tmp_next()
                P.add("act", f_act(tm[:, :], ph[:, :], AF.Identity, bias=cv(l, 16 + c)), reads=[kph, ("colv",)], writes=[ktm])
                P.add("dve", f_stt(cin[:, c, csl(s)], pc[:, :], cv(l, 12 + c), tm[:, :], ALU.add, ALU.mult),
                      reads=[kpc, ktm, ("colv",)], writes=[("cin", c, s)])
        ln_sub(1)
        for s in range(NSUB):
            for j in range(KSC):
                for c in range(4):
                    win = cin[:, c, 30 + j + s * SUB:30 + j + s * SUB + SUB]
                    wcol = cv(l, 172 + c * KSC + j)
                    if j == 0:
                        P.add("dve", f_ts(cacc[:, c, :], win, wcol, None, ALU.mult),
                              reads=cin_keys(c, s) + [("colv",)], writes=[("cacc", c)])
                    else:
                        P.add("dve", f_stt(cacc[:, c, :], win, wcol, cacc[:, c, :], ALU.mult, ALU.add),
                              reads=cin_keys(c, s) + [("cacc", c)], writes=[("cacc", c)])
            for c in range(4):
                P.add("dve", f_tt(bT[:, c, sl(s)], cacc[:, c, :], bT[:, c, sl(s)], ALU.mult),
                      reads=[("cacc", c), ("b", c, s)], writes=[("b", c, s)])
        save_halo(halo_c[l])
        for hf in range(2):
            wa, kwa = load_w(w_conf_out[l, :, hf * 512:(hf + 1) * 512], 4, 512)
            wbo, kwbo = load_w(w_sc_out[l, :, hf * 512:(hf + 1) * 512], 4, 512)
            wga, kwga = load_w(w_in[l, :, 2560 + hf * 512:2560 + (hf + 1) * 512], 8, 512)
            wgb, kwgb = load_w(w_in[l, :, 3584 + hf * 512:3584 + (hf + 1) * 512], 8, 512)
            for s in range(NSUB):
                for dq in range(4):
                    dc = hf * 4 + dq
                    q = slice(dq * 128, (dq + 1) * 128)
                    pga, kpga = ps_next()
                    P.add("pe", f_mm(pga[:, :], [(wga[:, k, q], xT[:, k, sl(s)]) for k in range(8)]),
                          reads=[kwga] + xT_keys(s), writes=[kpga])
                    pya, kpya = ps_next()
                    P.add("pe", f_mm(pya[:, :], [(wa[:, k, q], a2T[:, k, sl(s)]) for k in range(4)]),
                          reads=[kwa] + [("a2", k, s) for k in range(4)], writes=[kpya])
                    pgb, kpgb = ps_next()
                    P.add("pe", f_mm(pgb[:, :], [(wgb[:, k, q], xT[:, k, sl(s)]) for k in range(8)]),
                          reads=[kwgb] + xT_keys(s), writes=[kpgb])
                    pyb, kpyb = ps_next()
                    P.add("pe", f_mm(pyb[:, :], [(wbo[:, k, q], bT[:, k, sl(s)]) for k in range(4)]),
                          reads=[kwbo] + [("b", k, s) for k in range(4)], writes=[kpyb])
                    t1, kt1 = tmp_next()
                    t2, kt2 = tmp_next()
                    P.add("act", f_act(t1[:, :], pga[:, :], AF.Sigmoid, bias=cv(l, 20 + dc)), reads=[kpga, ("colv",)], writes=[kt1])
                    P.add("dve", f_tt(t1[:, :], t1[:, :], pya[:, :], ALU.mult), reads=[kt1, kpya], writes=[kt1])
                    P.add("act", f_act(t2[:, :], pgb[:, :], AF.Sigmoid, bias=cv(l, 28 + dc)), reads=[kpgb, ("colv",)], writes=[kt2])
                    P.add("dve", f_tt(t2[:, :], t2[:, :], pyb[:, :], ALU.mult), reads=[kt2, kpyb], writes=[kt2])
                    P.add("dve", f_tt(mT[:, dc, sl(s)], t1[:, :], t2[:, :], ALU.add), reads=[kt1, kt2], writes=[("m", dc, s)])
        wo = [load_w(w_o[l, :, hf * 512:(hf + 1) * 512], 8, 512) for hf in range(2)]
        load_ln(ln1_g, ln1_b, l)
        kt = (l == 1)

        def wo_tiles(tts):
            for tt in tts:
                for hf in range(2):
                    pz, kpz = ps_next()
                    P.add("pe", f_mm(pz[:, :], [(mT[:, k, tt * 128:(tt + 1) * 128], wo[hf][0][:, k, :]) for k in range(8)]),
                          reads=[wo[hf][1]] + [("m", k, tt // 4) for k in range(8)], writes=[kpz])
                    xs = xres[:, tt, hf * 512:(hf + 1) * 512]
                    P.add("dve", f_stt(xs, xs, ALPHA, pz[:, :], ALU.mult, ALU.add),
                          reads=[kpz, ("xres", tt, hf)], writes=[("xres", tt, hf)])
        wo_tiles(range(8) if kt else range(4))
        ln_compute([0, 1, 2, 3], kt)
        if not kt:
            wo_tiles(range(4, 8))
        ln_xpose([0, 1, 2, 3], kt)
        return {"c45": lambda: ln_compute([4, 5], kt), "c67": lambda: ln_compute([6, 7], kt),
                "x45": lambda: ln_xpose([4, 5], kt), "x67": lambda: ln_xpose([6, 7], kt)}

    router_bank = {}

    def router_logits(half):
        pl, kpl = ps_next()
        tts = range(half * 4, half * 4 + 4)

        def fn(pe):
            inst = None
            for tt in tts:
                for k in range(8):
                    inst = pe.matmul(pl[:, tt * NE:(tt + 1) * NE], xT[:, k, tt * 128:(tt + 1) * 128], wr[:, k, :],
                                     start=(k == 0), stop=False)
                inst = pe.matmul(pl[:, tt * NE:(tt + 1) * NE], ones_row[0:1, :], brow_r[0:1, :], start=False, stop=True)
            return inst
        P.add("pe", fn, reads=[("xT", tt) for tt in tts] + [("wr",), ("brow_r",), ("ones_row",)], writes=[kpl])
        cs = slice(half * 4 * NE, (half * 4 + 4) * NE)
        P.add("dve", f_copy(lgt[:, cs], pl[:, cs]), reads=[kpl], writes=[("lgt", half)])

    def router_tt(tt):
        pl, kpl = lgt[:, tt * NE:(tt + 1) * NE], ("lgt", tt // 4)
        s = sst[rr("sst", 4)]
        ks = ("sst", id(s))
        L, m1, mk1, L2, m2, mk2, dd, w1, w2 = (s[:, 16:24], s[:, 12:13], s[:, 24:32], s[:, 32:40], s[:, 13:14],
                                                 s[:, 40:48], s[:, 14:15], s[:, 15:16], s[:, 0:1])
        g = gate[:, tt, :]
        seq = [
            f_copy(L, pl),
            lambda e: e.tensor_reduce(out=m1, in_=L, axis=AX.X, op=ALU.max),
            f_ts(mk1, L, m1, None, ALU.is_equal),
            f_stt(L2, mk1, -1e30, L, ALU.mult, ALU.add),
            lambda e: e.tensor_reduce(out=m2, in_=L2, axis=AX.X, op=ALU.max),
            f_ts(mk2, L2, m2, None, ALU.is_equal),
            f_tt(dd, m1, m2, ALU.subtract),
        ]
        for i, fn in enumerate(seq):
            P.add("dve", fn, reads=[ks] + ([kpl] if i == 0 else []), writes=[ks])
        P.add("act", f_act(w1, dd, AF.Sigmoid), reads=[ks], writes=[ks])
        P.add("act", f_act(w2, dd, AF.Sigmoid, scale=-1.0), reads=[ks], writes=[ks])
        P.add("dve", f_tt(mskf[:, tt, :], mk1, mk2, ALU.add), reads=[ks], writes=[("mskf", tt)])
        P.add("dve", f_copy(msk[:, tt, :], mskf[:, tt, :]), reads=[("mskf", tt)], writes=[("msk", tt)])
        P.add("dve", f_ts(g, mk1, w1, None, ALU.mult), reads=[ks], writes=[("gate", tt)])
        P.add("dve", f_stt(g, mk2, w2, g, ALU.mult, ALU.add), reads=[ks, ("gate", tt)], writes=[("gate", tt)])

    def ranks():
        pr, kpr = ps_next()

        def fn(pe):
            inst = None
            for tt in range(NT):
                pairs = [(triu[:, :], msk[:, tt, :])] + [(ones_b[:, :], msk[:, t2, :]) for t2 in range(tt)]
                for i, (l_, r_) in enumerate(pairs):
                    inst = pe.matmul(pr[:, tt * NE:(tt + 1) * NE], l_, r_, start=(i == 0), stop=(i == len(pairs) - 1))
            return inst
        P.add("pe", fn, reads=[("triu",), ("ones_b",)] + [("msk", t2) for t2 in range(NT)], writes=[kpr])
        for tt in range(NT):
            P.add("dve", f_stt(rk[:, tt, :], pr[:, tt * NE:(tt + 1) * NE], 1.0, mskf[:, tt, :], ALU.add, ALU.mult),
                  reads=[kpr, ("mskf", tt)], writes=[("rk", tt)])
            P.add("dve", f_ts(rk[:, tt, :], rk[:, tt, :], -1.0, None, ALU.add), reads=[("rk", tt)], writes=[("rk", tt)])

    def moe_sparse():
        def S_ap(e, tt):
            buf = cacc_b if e % 2 == 0 else sqt_b
            return buf[:, tt // 2, (tt % 2) * 512:(tt % 2) * 512 + CAP]

        def S_key(e, tt):
            return ("cacc" if e % 2 == 0 else "sqt", tt // 2)

        def SeT_ap(jc):
            buf = lng_b if jc < 2 else lnb_b
            return buf[:, (jc % 2) * 1024:(jc % 2 + 1) * 1024]

        def SeT_key(jc):
            return ("lng",) if jc < 2 else ("lnb",)

        def xg_ap(k):
            return a2T[:, k // 2, (k % 2) * 512:(k % 2) * 512 + CAP]

        def xg_key(k):
            return ("a2", k // 2, k % 2)

        S_keys = lambda e: [S_key(e, tt) for tt in range(0, NT, 2)]

        def build_S(e):
            for tt in range(NT):
                P.add("dve", f_ts(S_ap(e, tt), iota_t[:, 0:CAP], rk[:, tt, e:e + 1], None, ALU.is_equal),
                      reads=[("rk", tt), ("iota",)], writes=[S_key(e, tt)])

        def gather(e):
            for k in range(8):
                pgt, kp = ps_next()
                P.add("pe", f_mm(pgt[:, 0:CAP], [(mT[:, tt, k * 128:(k + 1) * 128], S_ap(e, tt)) for tt in range(NT)]),
                      reads=[("m", tt, k // 4) for tt in range(NT)] + S_keys(e), writes=[kp])
                P.add("act", f_act(xg_ap(k), pgt[:, 0:CAP], AF.Copy), reads=[kp], writes=[xg_key(k)])

        def transp(e):
            for jc in range(NJ):
                pt, kpt = pst_next()
                P.add("pe", f_tr([(pt[:, tt * 128:(tt + 1) * 128], S_ap(e, tt)[:, jc * 128:(jc + 1) * 128])
                                  for tt in range(NT)], ident[:, :]),
                      reads=S_keys(e) + [("ident",)], writes=[kpt])
                P.add("act", f_act(SeT_ap(jc), pt[:, :], AF.Copy), reads=[kpt], writes=[SeT_key(jc)])

        def GU(wgv, kwg, wuv, kwu):
            hi = rr("h", 2)
            hbuf = hT[hi]
            for c in range(4):
                q = slice(c * 128, (c + 1) * 128)
                pg, kpg = ps_next()
                P.add("pe", f_mm(pg[:, 0:CAP], [(wgv[:, k, q], xg_ap(k)) for k in range(8)]),
                      reads=[kwg] + [xg_key(k) for k in range(8)], writes=[kpg])
                pu, kpu = ps_next()
                P.add("pe", f_mm(pu[:, 0:CAP], [(wuv[:, k, q], xg_ap(k)) for k in range(8)]),
                      reads=[kwu] + [xg_key(k) for k in range(8)], writes=[kpu])
                tm, ktm = tmp_next()
                P.add("act", f_act(tm[:, 0:CAP], pg[:, 0:CAP], AF.Silu), reads=[kpg], writes=[ktm])
                P.add("dve", f_tt(hbuf[:, c, 0:CAP], tm[:, 0:CAP], pu[:, 0:CAP], ALU.mult), reads=[ktm, kpu], writes=[("h", hi, c)])
            return hbuf, ("h", hi)

        def Dn(hbuf, hkey, wdv, kwd, first, last):
            for j4 in range(NJ):
                for hf in range(2):
                    hs = slice(hf * 512, (hf + 1) * 512)
                    pd, kpd = ps_next()
                    P.add("pe", f_mm(pd[:, :], [(hbuf[:, c, j4 * 128:(j4 + 1) * 128], wdv[:, c, hs]) for c in range(4)]),
                          reads=[kwd] + [hkey + (c,) for c in range(4)], writes=[kpd])
                    ya = cin[:, j4, HALO + hf * 512:HALO + (hf + 1) * 512]
                    yk = ("cin", j4, hf)
                    if first:
                        P.add("act", f_act(ya, pd[:, :], AF.Copy), reads=[kpd], writes=[yk])
                    elif last:
                        P.add("dve", f_tt(bT[:, j4, hs], ya, pd[:, :], ALU.add), reads=[kpd, yk], writes=[("b", j4, hf)])
                    else:
                        P.add("dve", f_tt(ya, ya, pd[:, :], ALU.add), reads=[kpd, yk], writes=[yk])

        def scatter(e):
            for tt in range(NT):
                for hf in range(2):
                    hs = slice(hf * 512, (hf + 1) * 512)
                    psc, kps = ps_next()
                    P.add("pe", f_mm(psc[:, :], [(SeT_ap(jc)[:, tt * 128:(tt + 1) * 128], bT[:, jc, hs]) for jc in range(NJ)]),
                          reads=[("lng",), ("lnb",)] + [("b", jc, hf) for jc in range(NJ)], writes=[kps])
                    xs = xres[:, tt, hs]
                    P.add("dve", f_stt(xs, psc[:, :], gate[:, tt, e:e + 1], xs, ALU.mult, ALU.add),
                          reads=[kps, ("xres", tt, hf), ("gate", tt)], writes=[("xres", tt, hf)])

        NFB = DEXP // 512
        build_S(0)
        gather(0)
        transp(0)
        for e in range(NE):
            prev = None
            for fb in range(NFB):
                cs = slice(fb * 512, (fb + 1) * 512)
                wgv, kwg = load_w_scr(sg[e][:, cs], 8, 512, e, sg)
                wuv, kwu = load_w_scr(su[e][:, cs], 8, 512, e, su)
                wdv, kwd = load_w_scr(sd[e][cs, :], 4, 1024, e, sd)
                hbuf, hkey = GU(wgv, kwg, wuv, kwu)
                if fb == 0 and e > 0:
                    scatter(e - 1)
                    transp(e)
                if prev is not None:
                    Dn(*prev, first=(fb == 1), last=False)
                if fb == 1 and e < NE - 1:
                    build_S(e + 1)
                prev = (hbuf, hkey, wdv, kwd)
            if e < NE - 1:
                gather(e + 1)
            Dn(*prev, first=False, last=True)
        scatter(NE - 1)

    def ple(l, b, pre=None, post=None):
        t0 = b * TB
        wpg = [load_w(w_ple_gate[l, :, hf * 512:(hf + 1) * 512], 8, 512) for hf in range(2)]
        wpp, kwpp = load_w(w_ple_proj[l, :, :], 2, 1024)
        P.add("pool", f_dma(brow_pg[:, :], b_ple_gate[0:1, l * D:(l + 1) * D]), writes=[("brow_pg",)], dma="bpg")
        for tt in range(NT):
            i = rr("ptok", 2)
            P.add("pool", f_dma(ptok[i][:, :], p_in[l, t0 + tt * 128:t0 + (tt + 1) * 128, :]),
                  writes=[("ptok", i)], dma=f"ptok{i}")
            transposes_to(ptok[i], ("ptok", i), 2, pT, ("pT", tt), tt)
        for tt in range(NT):
            for f in (pre or {}).get(tt, ()):
                f()
            tsl = slice(tt * 128, (tt + 1) * 128)
            for hf in range(2):
                hs = slice(hf * 512, (hf + 1) * 512)
                pg, kpg = ps_next()
                pairs = [(xT[:, k, tsl], wpg[hf][0][:, k, :]) for k in range(8)]
                pairs.append((ones_row[0:1, :], brow_pg[0:1, hf * 512:(hf + 1) * 512]))
                P.add("pe", f_mm(pg[:, :], pairs), reads=[("xT", tt), wpg[hf][1], ("brow_pg",), ("ones_row",)], writes=[kpg])
                pp, kpp = ps_next()
                P.add("pe", f_mm(pp[:, :], [(pT[:, q, tsl], wpp[:, q, hs]) for q in range(2)]),
                      reads=[("pT", tt), kwpp], writes=[kpp])
                tm, ktm = tmp_next()
                P.add("act", f_act(tm[:, :], pg[:, :], AF.Sigmoid), reads=[kpg], writes=[ktm])
                P.add("dve", f_tt(tm[:, :], tm[:, :], pp[:, :], ALU.mult), reads=[ktm, kpp], writes=[ktm])
                xs = xres[:, tt, hs]
                P.add("dve", f_stt(xs, xs, ALPHA, tm[:, :], ALU.mult, ALU.add),
                      reads=[ktm, ("xres", tt, hf)], writes=[("xres", tt, hf)])
            for f in (post or {}).get(tt, ()):
                f()

    def ffn(experts, before_last=None):
        items = []
        for (Wg, Wu, Wd, F, gcol) in experts:
            nchunks = F // 128
            f0 = 0
            while f0 < nchunks:
                nf = min(4, nchunks - f0)
                items.append((Wg, Wu, Wd, f0, nf, gcol))
                f0 += nf
        pend = []

        def emit_down(dn):
            (hbuf, hkey, wd, kwd, nf, gcol, s) = dn
            for t4 in range(4):
                tt = s * 4 + t4
                for hf in range(2):
                    pd, kpd = ps_next()
                    P.add("pe", f_mm(pd[:, :], [(hbuf[:, c, t4 * 128:(t4 + 1) * 128], wd[:, c, hf * 512:(hf + 1) * 512])
                                                 for c in range(nf)]),
                          reads=[kwd] + [hkey + (c,) for c in range(nf)], writes=[kpd])
                    xs = xres[:, tt, hf * 512:(hf + 1) * 512]
                    if gcol is None:
                        P.add("dve", f_tt(xs, xs, pd[:, :], ALU.add), reads=[kpd, ("xres", tt, hf)], writes=[("xres", tt, hf)])
                    else:
                        P.add("dve", f_stt(xs, pd[:, :], gate[:, tt, gcol:gcol + 1], xs, ALU.mult, ALU.add),
                              reads=[kpd, ("xres", tt, hf), ("gate", tt)], writes=[("xres", tt, hf)])

        for (Wg, Wu, Wd, f0, nf, gcol) in items:
            cs = slice(f0 * 128, (f0 + nf) * 128)
            wgv, kwg = load_w(Wg[:, cs], 8, nf * 128)
            wuv, kwu = load_w(Wu[:, cs], 8, nf * 128)
            wdv, kwd = load_w(Wd[cs, :], nf, 1024)
            for s in range(NSUB):
                hi = rr("h", 2)
                hbuf = hT[hi]
                hkey = ("h", hi)
                for c in range(nf):
                    q = slice(c * 128, (c + 1) * 128)
                    pg, kpg = ps_next()
                    P.add("pe", f_mm(pg[:, :], [(wgv[:, k, q], xT[:, k, s * SUB:(s + 1) * SUB]) for k in range(8)]),
                          reads=[kwg] + xT_keys(s), writes=[kpg])
                    pu, kpu = ps_next()
                    P.add("pe", f_mm(pu[:, :], [(wuv[:, k, q], xT[:, k, s * SUB:(s + 1) * SUB]) for k in range(8)]),
                          reads=[kwu] + xT_keys(s), writes=[kpu])
                    tm, ktm = tmp_next()
                    P.add("act", f_act(tm[:, :], pg[:, :], AF.Silu), reads=[kpg], writes=[ktm])
                    P.add("dve", f_tt(hbuf[:, c, :], tm[:, :], pu[:, :], ALU.mult), reads=[ktm, kpu], writes=[hkey + (c,)])
                pend.append((hbuf, hkey, wdv, kwd, nf, gcol, s))
                if len(pend) > 1:
                    emit_down(pend.pop(0))
        while pend:
            if len(pend) == 1 and before_last is not None:
                before_last()
            emit_down(pend.pop(0))

    last_l = layers[-1]
    first_l = layers[0]

    stage_bufs = [(xtok[i][:, :], [("xtok", i)], f"xtok{i}") for i in range(2)] + \
                 [(pT[:, q, :], [("pT", t) for t in range(NT)], f"ppT{q}") for q in range(2)] + \
                 [(tmps[i][:, :].bitcast(BF16), [("tmp", i)], f"ptmp{i}") for i in range(NTMP)]

    def prologue(b):
        t0 = b * TB
        loaded = []
        for tt in range(NT):
            buf, key, sem = stage_bufs[tt % len(stage_bufs)]
            P.add("pool", f_dma(buf, x[t0 + tt * 128:t0 + (tt + 1) * 128, :]), writes=key, dma=sem)
            loaded.append((buf, key))
        for tt, (bb, kk) in enumerate(loaded):
            pt, kpt = pst_next()
            P.add("pe", f_tr([(pt[:, k * 128:(k + 1) * 128], bb[:, k * 128:(k + 1) * 128]) for k in range(8)], ident[:, :]),
                  reads=kk + [("ident",)], writes=[kpt])
            P.add("act", f_act(xT[:, :, tt * 128:(tt + 1) * 128], pt[:, :].rearrange("p (k t) -> p k t", k=8), AF.Copy),
                  reads=[kpt], writes=[("xT", tt)])

    def load_x(b):
        t0 = b * TB
        P.add("sp", f_dma(xres[:, :, :], x[t0:t0 + TB, :].rearrange("(t p) d -> p t d", p=128)),
              writes=[("xres", tt, hf) for tt in range(NT) for hf in range(2)], dma="xin")

    def store_out(b):
        t0 = b * TB
        P.add("sp", f_dma(out[t0:t0 + TB, :].rearrange("(t p) d -> p t d", p=128), xres[:, :, :]),
              reads=[("xres", tt, hf) for tt in range(NT) for hf in range(2)], writes=[("out", b)], dma="out")

    prologue(0)
    mixer_A(first_l, 0)
    load_x(0)
    carry = None
    for b in range(n_blk):
        P.epoch = b
        for l in layers:
            if l == first_l:
                ln1_rest = mixer_rest(l, b, postB=carry)
                carry = None
            else:
                ln1_rest = mixer_rest(l, b)
            r = ln1_rest
            pre = {2: [r["c45"]], 3: [r["c67"]], 4: [r["x45"]], 6: [r["x67"]]}
            if l == 1:
                pre[4] = [r["x45"], partial(router_logits, 0)]
                pre[6] = [r["x67"], partial(router_logits, 1)]
                post = {4 + i: [partial(router_tt, 2 * i), partial(router_tt, 2 * i + 1)] for i in range(4)}
                ple(l, b, pre=pre, post=post)
                ranks()
                moe_sparse()
                load_ln(ln2_g, ln2_b, l)
            else:
                ple(l, b, pre=pre)
                load_ln(ln2_g, ln2_b, l)
                ffn([(w_ff_gate[0], w_ff_up[0], w_ff_down[0], DFF, None)],
                    before_last=(lambda: ln_compute([0, 1, 2, 3])) if l != last_l else None)
            if l != last_l:
                nl = layers[layers.index(l) + 1]
                ln_xpose([0, 1, 2, 3])
                mixer_A(nl, b, post={(0, 0): [lambda: ln_compute([4, 5])], (0, 1): [lambda: ln_compute([6, 7])],
                                     (0, 3): [lambda: ln_xpose([4, 5, 6, 7])]})
            elif b + 1 < n_blk:
                prologue(b + 1)
                mixer_A(first_l, b + 1)
                carry = {0: [lambda: ln_group([0, 1, 2, 3], False)],
                         1: [lambda: ln_group([4, 5, 6, 7], False), partial(store_out, b), partial(load_x, b + 1)]}
            else:
                layer_norm_all(False)
                store_out(b)
    P.add("sp", None, reads=[("out", b) for b in range(n_blk)])

    P.emit(nc, es)
    es.close()
    return nc


def _colvecs(inp):
    cols = np.zeros((128, 2 * NCOLV), np.float32)
    for l in range(2):
        base = l * NCOLV
        cols[:, base:base + 36] = np.asarray(inp["b_in"][l]).reshape(36, 128).T
        cw = np.asarray(inp["conf_conv_w"][l]).reshape(KCONF, 4, 128)
        cols[:, base + 36:base + 160] = cw.transpose(2, 1, 0).reshape(128, 4 * KCONF)
        cols[:, base + 160:base + 164] = np.asarray(inp["conf_conv_b"][l]).reshape(4, 128).T
        cols[:, base + 164:base + 168] = np.asarray(inp["conf_ln_g"][l]).reshape(4, 128).T
        cols[:, base + 168:base + 172] = np.asarray(inp["conf_ln_b"][l]).reshape(4, 128).T
        sw = np.asarray(inp["sc_conv_w"][l]).reshape(KSC, 4, 128)
        cols[:, base + 172:base + 184] = sw.transpose(2, 1, 0).reshape(128, 4 * KSC)
    return cols


_NC_CACHE = {}


def _get_nc(n_blk, layers):
    key = (n_blk, tuple(layers))
    if key not in _NC_CACHE:
        _NC_CACHE[key] = build_nc(n_blk, layers)
    return _NC_CACHE[key]


def make_in_maps(inp, cores):
    f = lambda a: np.ascontiguousarray(np.asarray(a, dtype=np.float32))
    shared = {k: f(inp[k]) for k in ("w_in", "w_conf_out", "w_sc_out", "w_o", "w_ff_gate", "w_ff_up", "w_ff_down",
                                     "w_router", "b_router", "we_gate", "we_up", "we_down", "w_ple_gate",
                                     "w_ple_proj", "ln1_g", "ln1_b", "ln2_g", "ln2_b")}
    shared["b_ple_gate"] = f(inp["b_ple_gate"]).reshape(1, 2 * D)
    shared["colv"] = _colvecs(inp)
    shared["ident"] = np.eye(128, dtype=np.float32)
    shared["iota"] = np.ascontiguousarray(np.broadcast_to(np.arange(512, dtype=np.float32), (128, 512)))
    shared["triu"] = np.triu(np.ones((128, 128), np.float32), k=1)
    xs = np.asarray(inp["x"], dtype=np.float32)
    ps_ = np.asarray(inp["p"], dtype=np.float32)
    maps = []
    for c in cores:
        m = dict(shared)
        m["x"] = np.ascontiguousarray(xs[c])
        m["p"] = np.ascontiguousarray(ps_[:, c])
        maps.append(m)
    return maps


def kernel(**inputs):
    nc = _get_nc(NBLK, (0, 1))
    in_maps = make_in_maps(inputs, list(range(NCORES)))
    res = run_bass_kernel_spmd(nc, in_maps, core_ids=list(range(NCORES)))
    return np.stack([np.asarray(r["out"], dtype=np.float32) for r in res.results], axis=0)
```
